# Optimizing a Trainium2 kernel written in Bass

```python
import jax, jax.numpy as jnp
from jax import lax
import numpy as np

D_MODEL = 1024
BATCH = 8
SEQ = 2048
DEPTH = 1

CONV_DIM = 512
CONV_WIDTH = 31
GLA_HEADS = 4
GLA_DK = 128
GLA_DV = 256
GLA_LOWRANK = 16
GLA_TAU = 16.0
GLA_CHUNK = 64
QK_DIM = GLA_HEADS * GLA_DK
V_DIM = GLA_HEADS * GLA_DV
N_BRANCH = 2
N_GROUPS = 4
EXPERTS_PER_GROUP = 8
N_EXPERTS = N_GROUPS * EXPERTS_PER_GROUP
TOP_K = 2
D_EXPERT = 512
EPS = 1e-6
N_MOD = 6
IN_SIZES = (2 * CONV_DIM, QK_DIM, QK_DIM, V_DIM, V_DIM, GLA_LOWRANK, N_BRANCH * D_MODEL)
IN_DIM = 2 * CONV_DIM + 2 * QK_DIM + 2 * V_DIM + GLA_LOWRANK + N_BRANCH * D_MODEL

kernel_name = "hybrid_conv_gla_hmoe_adaln_block"


def rmsnorm(x, g):
    x32 = x.astype(jnp.float32)
    y = x32 * lax.rsqrt(jnp.mean(x32 * x32, axis=-1, keepdims=True) + EPS)
    return (y * g.astype(jnp.float32)).astype(x.dtype)


def layernorm(x, g, b):
    x32 = x.astype(jnp.float32)
    mu = jnp.mean(x32, axis=-1, keepdims=True)
    xc = x32 - mu
    var = jnp.mean(xc * xc, axis=-1, keepdims=True)
    y = xc * lax.rsqrt(var + EPS) * g.astype(jnp.float32) + b.astype(jnp.float32)
    return y.astype(x.dtype)


def modulate(x, shift, scale):
    return x * (1.0 + scale[:, None, :]) + shift[:, None, :]


def conv_module(u, w_dw, b_dw, ln_g, ln_b, w_pw, b_pw):
    a, gt = jnp.split(u, 2, axis=-1)
    z = a * jax.nn.sigmoid(gt)
    z = lax.conv_general_dilated(
        z, w_dw, window_strides=(1,), padding=[(CONV_WIDTH - 1, 0)],
        dimension_numbers=("NWC", "WIO", "NWC"), feature_group_count=CONV_DIM) + b_dw
    z = jax.nn.silu(layernorm(z, ln_g, ln_b))
    return z @ w_pw + b_pw


def gla_chunked(q, k, v, logg):
    B, S, H, _ = q.shape
    n_chunks = S // GLA_CHUNK

    def to_chunks(t):
        return t.reshape(B, n_chunks, GLA_CHUNK, H, t.shape[-1]).transpose(1, 0, 3, 2, 4)

    causal = jnp.tril(jnp.ones((GLA_CHUNK, GLA_CHUNK), dtype=bool))[:, :, None]

    def step(state, inp):
        qc, kc, vc, gc = inp
        b = jnp.cumsum(gc, axis=2)
        o_inter = jnp.einsum("bhcd,bhde->bhce", qc * jnp.exp(b), state)
        diff = b[:, :, :, None, :] - b[:, :, None, :, :]
        decay = jnp.exp(jnp.where(causal, diff, -jnp.inf))
        attn = jnp.einsum("bhid,bhijd,bhjd->bhij", qc, decay, kc)
        o_intra = jnp.einsum("bhij,bhje->bhie", attn, vc)
        b_last = b[:, :, -1:, :]
        new_state = (jnp.exp(b_last[:, :, 0, :])[..., None] * state
                     + jnp.einsum("bhcd,bhce->bhde", kc * jnp.exp(b_last - b), vc))
        return new_state, o_inter + o_intra

    state0 = jnp.zeros((B, H, q.shape[-1], v.shape[-1]), jnp.float32)
    _, outs = lax.scan(step, state0, (to_chunks(q), to_chunks(k), to_chunks(v), to_chunks(logg)))
    return outs.transpose(1, 0, 3, 2, 4).reshape(B, S, H, v.shape[-1])


def hybrid_mixer(u, w_in, w_dw, b_dw, g_conv_ln, b_conv_ln, w_conv_pw, b_conv_pw,
                 w_a2, b_a2, g_gla_norm, w_gla_o, w_out):
    B, S, _ = u.shape
    proj = u @ w_in
    offs = np.cumsum(IN_SIZES)[:-1].tolist()
    p_conv, q, k, v, r, a1, gates = jnp.split(proj, offs, axis=-1)
    y_conv = conv_module(p_conv, w_dw, b_dw, g_conv_ln, b_conv_ln, w_conv_pw, b_conv_pw)
    q = q.reshape(B, S, GLA_HEADS, GLA_DK).astype(jnp.float32) * (GLA_DK ** -0.5)
    k = k.reshape(B, S, GLA_HEADS, GLA_DK).astype(jnp.float32)
    v = v.reshape(B, S, GLA_HEADS, GLA_DV).astype(jnp.float32)
    logg = jax.nn.log_sigmoid((a1 @ w_a2 + b_a2).astype(jnp.float32)) / GLA_TAU
    logg = logg.reshape(B, S, GLA_HEADS, GLA_DK)
    o = gla_chunked(q, k, v, logg)
    o = rmsnorm(o, g_gla_norm.reshape(GLA_HEADS, GLA_DV)).reshape(B, S, V_DIM).astype(u.dtype)
    y_gla = (o * jax.nn.silu(r)) @ w_gla_o
    gt = jax.nn.sigmoid(gates).reshape(B, S, N_BRANCH, D_MODEL)
    merged = gt[:, :, 0, :] * y_conv + gt[:, :, 1, :] * y_gla
    return merged @ w_out


def hierarchical_moe(h, w_rg, b_rg, w_re, b_re, w1, w3, w2):
    B, S, D = h.shape
    t = h.reshape(B * S, D)
    lg = (t @ w_rg + b_rg).astype(jnp.float32)
    pg = jax.nn.softmax(lg, axis=-1)
    _, gsel = lax.top_k(lg, 1)
    wg = jnp.take_along_axis(pg, gsel, axis=1)
    le = (t @ w_re + b_re).astype(jnp.float32).reshape(-1, N_GROUPS, EXPERTS_PER_GROUP)
    le_sel = jnp.take_along_axis(le, gsel[:, :, None], axis=1)[:, 0, :]
    top_v, top_i = lax.top_k(le_sel, TOP_K)
    w = jax.nn.softmax(top_v, axis=-1) * wg
    eid = gsel * EXPERTS_PER_GROUP + top_i
    comb = jnp.einsum("tk,tke->te", w, jax.nn.one_hot(eid, N_EXPERTS, dtype=jnp.float32))
    y = jnp.zeros((B * S, D), jnp.float32)
    for e in range(N_EXPERTS):
        hid = jax.nn.silu(t @ w1[e]) * (t @ w3[e])
        y = y + comb[:, e:e + 1] * (hid @ w2[e]).astype(jnp.float32)
    return y.astype(h.dtype).reshape(B, S, D)


def setup_inputs(seed: int = 0) -> dict:
    key = jax.random.key(seed)
    ks = jax.random.split(key, 32)
    f32 = jnp.float32
    L, D = DEPTH, D_MODEL

    def nrm(k, shape, scale):
        return jax.random.normal(k, shape, f32) * scale

    def gain(k, shape):
        return 1.0 + 0.05 * jax.random.normal(k, shape, f32)

    return {
        "x": jax.random.normal(ks[0], (BATCH, SEQ, D), f32),
        "c": jax.random.normal(ks[1], (BATCH, D), f32),
        "w_ada": nrm(ks[2], (L, D, N_MOD * D), 0.5 * D ** -0.5),
        "b_ada": nrm(ks[3], (L, N_MOD * D), 0.02),
        "g_norm1": gain(ks[4], (L, D)),
        "w_in": nrm(ks[5], (L, D, IN_DIM), D ** -0.5),
        "w_dw": nrm(ks[6], (L, CONV_WIDTH, 1, CONV_DIM), CONV_WIDTH ** -0.5),
        "b_dw": nrm(ks[7], (L, CONV_DIM), 0.02),
        "g_conv_ln": gain(ks[8], (L, CONV_DIM)),
        "b_conv_ln": nrm(ks[9], (L, CONV_DIM), 0.02),
        "w_conv_pw": nrm(ks[10], (L, CONV_DIM, D), CONV_DIM ** -0.5),
        "b_conv_pw": nrm(ks[11], (L, D), 0.02),
        "w_a2": nrm(ks[12], (L, GLA_LOWRANK, QK_DIM), GLA_LOWRANK ** -0.5),
        "b_a2": nrm(ks[13], (L, QK_DIM), 0.02),
        "g_gla_norm": gain(ks[14], (L, V_DIM)),
        "w_gla_o": nrm(ks[15], (L, V_DIM, D), V_DIM ** -0.5),
        "w_out": nrm(ks[16], (L, D, D), D ** -0.5),
        "g_norm2": gain(ks[17], (L, D)),
        "w_router_g": nrm(ks[18], (L, D, N_GROUPS), D ** -0.5),
        "b_router_g": nrm(ks[19], (L, N_GROUPS), 0.01),
        "w_router_e": nrm(ks[20], (L, D, N_EXPERTS), D ** -0.5),
        "b_router_e": nrm(ks[21], (L, N_EXPERTS), 0.01),
        "w1": nrm(ks[22], (L, N_EXPERTS, D, D_EXPERT), D ** -0.5),
        "w3": nrm(ks[23], (L, N_EXPERTS, D, D_EXPERT), D ** -0.5),
        "w2": nrm(ks[24], (L, N_EXPERTS, D_EXPERT, D), D_EXPERT ** -0.5),
        "w_ada_f": nrm(ks[25], (D, 2 * D), 0.5 * D ** -0.5),
        "b_ada_f": nrm(ks[26], (2 * D,), 0.02),
        "g_final": gain(ks[27], (D,)),
    }


def reference(x, c, w_ada, b_ada, g_norm1, w_in, w_dw, b_dw, g_conv_ln, b_conv_ln,
              w_conv_pw, b_conv_pw, w_a2, b_a2, g_gla_norm, w_gla_o, w_out, g_norm2,
              w_router_g, b_router_g, w_router_e, b_router_e, w1, w3, w2,
              w_ada_f, b_ada_f, g_final):
    h = x
    c_act = jax.nn.silu(c)
    for l in range(DEPTH):
        mod = c_act @ w_ada[l] + b_ada[l]
        sh1, sc1, ga1, sh2, sc2, ga2 = jnp.split(mod, N_MOD, axis=-1)
        u = modulate(rmsnorm(h, g_norm1[l]), sh1, sc1)
        y = hybrid_mixer(u, w_in[l], w_dw[l], b_dw[l], g_conv_ln[l], b_conv_ln[l],
                         w_conv_pw[l], b_conv_pw[l], w_a2[l], b_a2[l], g_gla_norm[l],
                         w_gla_o[l], w_out[l])
        h = h + ga1[:, None, :] * y
        u2 = modulate(rmsnorm(h, g_norm2[l]), sh2, sc2)
        y2 = hierarchical_moe(u2, w_router_g[l], b_router_g[l], w_router_e[l], b_router_e[l],
                              w1[l], w3[l], w2[l])
        h = h + ga2[:, None, :] * y2
    modf = c_act @ w_ada_f + b_ada_f
    shf, scf = jnp.split(modf, 2, axis=-1)
    return modulate(rmsnorm(h, g_final), shf, scf)
```

```python
import numpy as np
from contextlib import ExitStack
import concourse.bass as bass
import concourse.mybir as mybir
from concourse.bass_utils import run_bass_kernel_spmd

F32 = mybir.dt.float32
BF16 = mybir.dt.bfloat16
AF = mybir.ActivationFunctionType
ALU = mybir.AluOpType
AX = mybir.AxisListType

EPS = 1e-6
NT = 16
NQ = 4
NE = 32
CAP = 256
NOT = 30
NSLOT = NE * CAP + NOT * 128
I32 = mybir.dt.int32


class Op:
    __slots__ = ("eng", "fn", "deps", "sig", "sigval", "dsem", "dval")

    def __init__(self, eng, fn):
        self.eng = eng
        self.fn = fn
        self.deps = []
        self.sig = False
        self.sigval = 0
        self.dsem = None
        self.dval = 0


class Prog:
    ENG = ("pe", "act", "dve", "pool", "sp")

    def __init__(self, nc, es):
        self.nc = nc
        self.es = es
        self.ops = {e: [] for e in self.ENG}
        self.res = {}
        self.dsems = {}
        self.esem = {}
        for e in self.ENG:
            self.esem[e] = es.enter_context(nc.semaphore("es_" + e))
        self.final = []
        self.bank = 0

    def nb(self):
        b = self.bank
        self.bank = (self.bank + 1) % 8
        return b

    def add(self, eng, fn, r=(), w=(), dsem=None):
        o = Op(eng, fn)
        deps = []
        for x in r:
            st = self.res.get(x)
            if st is not None and st[0] is not None:
                deps.append(st[0])
        for x in w:
            st = self.res.get(x)
            if st is not None:
                if st[0] is not None:
                    deps.append(st[0])
                deps.extend(st[1])
        for x in r:
            st = self.res.get(x)
            if st is None:
                st = [None, []]
                self.res[x] = st
            st[1].append(o)
        for x in w:
            self.res[x] = [o, []]
        seen = set()
        for d in deps:
            if d is o or id(d) in seen:
                continue
            seen.add(id(d))
            if d.eng == "pe" and eng == "pe" and d.dsem is None:
                continue
            o.deps.append(d)
            d.sig = True
        if dsem is not None:
            if dsem not in self.dsems:
                self.dsems[dsem] = [self.es.enter_context(self.nc.semaphore("ds_" + dsem)), 0, None]
            ent = self.dsems[dsem]
            ent[1] += 16
            ent[2] = o
            o.dsem = dsem
            o.dval = ent[1]
        self.ops[eng].append(o)
        return o

    def barrier(self, keep=()):
        lasts = []
        for e in self.ENG:
            for o in reversed(self.ops[e]):
                if o.dsem is None and o.fn is not None:
                    lasts.append(o)
                    break
        for name, ent in self.dsems.items():
            if ent[2] is not None and name not in keep:
                lasts.append(ent[2])
        for o in lasts:
            o.sig = True
        for e in self.ENG:
            b = Op(e, None)
            b.deps = list(lasts)
            self.ops[e].append(b)
        self.res = {k: v for k, v in self.res.items() if k in keep}

    def emit(self, block):
        for e in self.ENG:
            c = 0
            for o in self.ops[e]:
                if o.sig and o.dsem is None and o.fn is not None:
                    c += 1
                    o.sigval = c
        me = self

        def run(engname, eng):
            waited = {}
            for o in me.ops[engname]:
                for d in o.deps:
                    if d.dsem is not None:
                        key = "d_" + d.dsem
                        sem, val = me.dsems[d.dsem][0], d.dval
                    else:
                        key = "e_" + d.eng
                        sem, val = me.esem[d.eng], d.sigval
                    if waited.get(key, 0) < val:
                        eng.wait_ge(sem, val)
                        waited[key] = val
                if o.fn is None:
                    continue
                ins = o.fn(eng)
                if o.dsem is not None:
                    ins.then_inc(me.dsems[o.dsem][0], 16)
                elif o.sig:
                    ins.then_inc(me.esem[engname], 1)
            for (fe, name) in me.final:
                if fe == engname:
                    eng.wait_ge(me.dsems[name][0], me.dsems[name][1])

        block.tensor(lambda e: run("pe", e))
        block.scalar(lambda e: run("act", e))
        block.vector(lambda e: run("dve", e))
        block.gpsimd(lambda e: run("pool", e))
        block.sync(lambda e: run("sp", e))


class Arena:
    def __init__(self, nc, es, nwords, name="arena"):
        self.t = es.enter_context(nc.sbuf_tensor(name, [128, nwords], F32))
        self.n = nwords
        self.off = 0
        self.hi = 0

    def mark(self):
        return self.off

    def release(self, m):
        self.off = m

    def alloc(self, shape, dtype):
        free = 1
        for s in shape[1:]:
            free *= s
        words = (free + 1) // 2 if dtype == BF16 else free
        words = (words + 7) // 8 * 8
        assert self.off + words <= self.n, f"arena overflow {self.off}+{words}>{self.n}"
        ap = self.t[0:shape[0], self.off:self.off + words]
        self.off += words
        self.hi = max(self.hi, self.off)
        if dtype == BF16:
            ap = ap.bitcast(BF16)[:, 0:free]
        elif dtype == F32:
            ap = ap[:, 0:free]
        else:
            ap = ap.bitcast(dtype)[:, 0:free]
        if len(shape) == 3:
            ap = ap.rearrange("p (a b) -> p a b", a=shape[1])
        elif len(shape) == 4:
            ap = ap.rearrange("p (a b c) -> p a b c", a=shape[1], b=shape[2])
        return ap


def build_program(debug=None, stop_after=None):
    debug = debug or []
    nc = bass.Bass("TRN2", target_bir_lowering=False)

    def din(name, shape):
        return nc.dram_tensor(name, shape, F32, kind="ExternalInput").ap()

    x = din("x", [2048, 1024])
    c = din("c", [8, 128])
    w_ada = din("w_ada", [1024, 6144])
    b_ada = din("b_ada", [1, 6144])
    g_norm1 = din("g_norm1", [1, 1024])
    w_in = din("w_in", [1024, 6160])
    w_dw = din("w_dw", [31, 512])
    b_dw = din("b_dw", [4, 128])
    g_conv_ln = din("g_conv_ln", [4, 128])
    b_conv_ln = din("b_conv_ln", [4, 128])
    w_conv_pw = din("w_conv_pw", [512, 1024])
    b_conv_pw = din("b_conv_pw", [8, 128])
    w_a2 = din("w_a2", [16, 512])
    b_a2 = din("b_a2", [1, 512])
    g_gla_norm = din("g_gla_norm", [1, 1024])
    w_gla_o = din("w_gla_o", [1024, 1024])
    w_out = din("w_out", [1024, 1024])
    g_norm2 = din("g_norm2", [1, 1024])
    w_router_g = din("w_router_g", [1024, 4])
    b_router_g = din("b_router_g", [1, 4])
    w_router_e = din("w_router_e", [1024, 32])
    b_router_e = din("b_router_e", [1, 32])
    wpack = din("wpack", [NE * 128, 12288])
    w_ada_f = din("w_ada_f", [1024, 2048])
    b_ada_f = din("b_ada_f", [1, 2048])
    g_final = din("g_final", [1, 1024])
    out = nc.dram_tensor("out", [2048, 1024], F32, kind="ExternalOutput").ap()
    h_scr = nc.dram_tensor("h_scr", [2048, 1024], F32).ap()
    u2_scr = nc.dram_tensor("u2_scr", [2048, 1024], BF16).ap()
    X_scr = nc.dram_tensor("X_scr", [NSLOT, 1024], BF16).ap()
    Y_scr = nc.dram_tensor("Y_scr", [NSLOT, 1024], F32).ap()
    dbg_outs = {}

    with ExitStack() as es:
        P = Prog(nc, es)
        A = Arena(nc, es, 52800)
        ps = es.enter_context(nc.psum_tensor("ps", [128, 8, 512], F32))

        def bankf(b):
            return ps[:, b, :]

        def bankb(b):
            return ps[:, b, :].bitcast(BF16)

        def MM(out_ap, pairs, r, w):
            def fn(e):
                n = len(pairs)
                ins = None
                for i, (l, rh) in enumerate(pairs):
                    ins = e.matmul(out_ap, lhsT=l, rhs=rh, start=(i == 0), stop=(i == n - 1))
                return ins
            return P.add("pe", fn, r=r, w=w)

        def TR(outs_ins, ident_ap, r, w):
            def fn(e):
                ins = None
                for (o, i) in outs_ins:
                    ins = e.transpose(out=o, in_=i, identity=ident_ap)
                return ins
            return P.add("pe", fn, r=r, w=w)

        def ACT(out_ap, in_ap, func, r, w, **kw):
            return P.add("act", lambda e: e.activation(out=out_ap, in_=in_ap, func=func, **kw), r=r, w=w)

        def TT(eng, out_ap, in0, in1, op, r, w):
            return P.add(eng, lambda e: e.tensor_tensor(out=out_ap, in0=in0, in1=in1, op=op), r=r, w=w)

        def TS(eng, out_ap, in0, s1, s2, op0, op1, r, w):
            return P.add(eng, lambda e: e.tensor_scalar(out=out_ap, in0=in0, scalar1=s1, scalar2=s2, op0=op0, op1=op1), r=r, w=w)

        def TS1(eng, out_ap, in0, s1, op, r, w):
            return P.add(eng, lambda e: e.tensor_single_scalar(out=out_ap, in_=in0, scalar=s1, op=op), r=r, w=w)

        def STT(eng, out_ap, in0, scalar, in1, op0, op1, r, w):
            return P.add(eng, lambda e: e.scalar_tensor_tensor(out=out_ap, in0=in0, scalar=scalar, in1=in1, op0=op0, op1=op1), r=r, w=w)

        def CP(eng, out_ap, in_ap, r, w):
            return P.add(eng, lambda e: e.tensor_copy(out=out_ap, in_=in_ap), r=r, w=w)

        def MS(eng, ap, val, w):
            return P.add(eng, lambda e: e.memset(ap, val), w=w)

        def DMA(eng, out_ap, in_ap, r, w, dsem):
            return P.add(eng, lambda e: e.dma_start(out=out_ap, in_=in_ap), r=r, w=w, dsem=dsem)

        def RSTD(out_ap, ss_ap, n, r, w):
            ACT(out_ap, ss_ap, AF.Ln, r=r, w=w, scale=1.0 / n, bias=EPS)
            ACT(out_ap, out_ap, AF.Exp, r=w, w=w, scale=-0.5)

        def DBG(name, ap, shape, r):
            if name not in debug:
                return
            d = nc.dram_tensor("dbg_" + name, shape, F32, kind="ExternalOutput").ap()
            dbg_outs[name] = d
            DMA("pool", d, ap, r=r, w=["dbg_" + name], dsem="dbg")

        modB = A.alloc([128, 3, 1024], F32)
        _mod = {}

        def mod(m, lo=0, hi=1024):
            return (_mod["A"][:, m, lo:hi] if m < 5 else modB[:, m - 5, lo:hi])
        logits = A.alloc([128, NT, 36], F32)
        ss3 = A.alloc([128, NT], F32)
        rstd3 = A.alloc([128, NT], F32)
        identb = A.alloc([128, 128], BF16)
        ones_bf = A.alloc([128, 128], BF16)
        phase_mark = A.mark()
        _mod["A"] = A.alloc([128, 5, 1024], F32)
        identf = A.alloc([128, 128], F32)
        mask4 = A.alloc([128, 4, 128], F32)
        resetm = A.alloc([128, 512], F32)
        vecs = A.alloc([128, 28], F32)
        wdwT = A.alloc([128, 4, 32], F32)
        cT = A.alloc([128, 8], BF16)
        crep = A.alloc([128, 8, 128], BF16)
        wa1 = A.alloc([128, 8, 16], BF16)
        wa2aug = A.alloc([17, 512], BF16)
        a1T = A.alloc([17, 512], BF16)
        g_gla_bc = A.alloc([128, 1024], F32)
        wr_bf = A.alloc([128, 8, 36], BF16)
        br_bc = A.alloc([128, 36], F32)
        ss1 = A.alloc([128, NT], F32)
        rstd1 = A.alloc([128, NT], F32)
        ss2 = A.alloc([128, NT], F32)
        rstd2 = A.alloc([128, NT], F32)
        zT = A.alloc([128, 4, 542], BF16)
        S32 = A.alloc([128, 4, 256], F32)
        S16 = A.alloc([128, 4, 256], BF16)
        stage_w = A.alloc([31, 512], F32)
        stage_v = A.alloc([28, 128], F32)

        MS("pool", identf, 0.0, w=["identf"])
        P.add("pool", lambda e: e.affine_select(out=identf, in_=identf, pattern=[[-1, 128]], compare_op=ALU.not_equal,
                                                fill=1.0, base=0, channel_multiplier=1), r=["identf"], w=["identf"])
        CP("pool", identb, identf, r=["identf"], w=["identb"])
        MS("pool", ones_bf, 1.0, w=["ones_bf"])
        MS("pool", mask4, 1.0, w=["mask4"])
        P.add("pool", lambda e: e.affine_select(out=mask4, in_=mask4, pattern=[[0, 4], [1, 128]], compare_op=ALU.is_ge,
                                                fill=0.0, base=0, channel_multiplier=-1), r=["mask4"], w=["mask4"])
        MS("pool", resetm, 1.0, w=["resetm"])
        for t in range(4):
            MS("pool", resetm[:, t * 128:t * 128 + 1], 0.0, w=["resetm"])
        MS("pool", a1T, 1.0, w=["a1T"])
        MS("pool", zT, 0.0, w=["zT"])

        DMA("sp", stage_w, w_dw[:, :], r=[], w=["stage_w"], dsem="c_sw")
        DMA("sp", stage_v[0:4, :], b_dw[:, :], r=[], w=["stage_v"], dsem="c_sv")
        DMA("sp", stage_v[4:8, :], g_conv_ln[:, :], r=[], w=["stage_v"], dsem="c_sv")
        DMA("sp", stage_v[8:12, :], b_conv_ln[:, :], r=[], w=["stage_v"], dsem="c_sv")
        DMA("sp", stage_v[12:20, :], b_conv_pw[:, :], r=[], w=["stage_v"], dsem="c_sv")
        DMA("sp", stage_v[20:28, :], c[:, :], r=[], w=["stage_v"], dsem="c_sv")
        DMA("sp", g_gla_bc, g_gla_norm.partition_broadcast(128), r=[], w=["g_gla_bc"], dsem="c_gg")
        DMA("sp", br_bc[:, 0:4], b_router_g.partition_broadcast(128), r=[], w=["br_bc"], dsem="c_br")
        DMA("sp", br_bc[:, 4:36], b_router_e.partition_broadcast(128), r=[], w=["br_bc"], dsem="c_br")
        with nc.allow_non_contiguous_dma(reason="small router/a1 weights"):
            DMA("pool", wa1, w_in[:, 4096:4112].rearrange("(k p) f -> p k f", p=128), r=[], w=["wa1"], dsem="c_wa1")
            DMA("pool", wr_bf[:, :, 0:4], w_router_g.rearrange("(k p) f -> p k f", p=128), r=[], w=["wr_bf"], dsem="c_wr")
            DMA("pool", wr_bf[:, :, 4:36], w_router_e.rearrange("(k p) f -> p k f", p=128), r=[], w=["wr_bf"], dsem="c_wr")
        DMA("pool", wa2aug[0:16, :], w_a2[:, :], r=[], w=["wa2aug"], dsem="c_wa2")
        DMA("pool", wa2aug[16:17, :], b_a2[:, :], r=[], w=["wa2aug"], dsem="c_wa2")


        NWB = 3
        wbuf = [A.alloc([128, 8, 512], BF16) for _ in range(NWB)]
        junk = A.alloc([128, 1024], BF16)
        tmpA = A.alloc([128, 1024], F32)
        ut = [A.alloc([128, 1024], BF16) for _ in range(2)]
        uT_q = A.alloc([128, 8, 512], BF16)
        m0T = A.alloc([128, 8, 512], BF16)
        qT = A.alloc([128, 4, 512], BF16)
        kT = A.alloc([128, 4, 512], BF16)
        v_q = A.alloc([128, 4, 1024], BF16)
        sr_q = A.alloc([128, 4, 1024], BF16)
        g0s = A.alloc([128, 8, 512], BF16)
        ebl = A.alloc([128, 4, 4], F32)
        oss = A.alloc([128, 4], F32)
        orstd = A.alloc([128, 4], F32)
        og = [A.alloc([128, 1024], BF16) for _ in range(2)]
        ogT_q = A.alloc([128, 8, 512], BF16)
        xr = [A.alloc([128, 1024], F32) for _ in range(2)]
        xt = xr[0]
        attnT = A.alloc([128, 4, 128], BF16)
        ztile = A.alloc([128, 512], BF16)
        zc = A.alloc([128, 4, 512], BF16)
        zsq = A.alloc([128, 4, 512], BF16)
        diag = A.alloc([128, 31, 128], BF16)
        sgt = [A.alloc([128, 512], BF16) for _ in range(2)]
        mean_sb = A.alloc([128, 512], F32)
        rstd_sb = A.alloc([128, 512], F32)
        lnt = A.alloc([128, 512], F32)
        klT = A.alloc([128, 4, 512], BF16)
        kl = A.alloc([128, 4, 4, 128], BF16)
        gX = [A.alloc([128, 512], F32) for _ in range(2)]
        gY = [A.alloc([128, 512], F32) for _ in range(2)]
        print("arena words used (mixer phase):", A.mark())

        sw_hi, sw_lo = junk[0:31, 0:512], junk[0:31, 512:1024]
        sv_hi, sv_lo = og[0][0:28, 0:128], og[0][0:28, 128:256]
        CP("dve", sw_hi, stage_w, r=["stage_w"], w=["junk"])
        TT("dve", sw_lo, stage_w, sw_hi, ALU.subtract, r=["stage_w", "junk"], w=["junk"])
        CP("dve", sv_hi, stage_v, r=["stage_v"], w=["og0"])
        TT("dve", sv_lo, stage_v, sv_hi, ALU.subtract, r=["stage_v", "og0"], w=["og0"])
        bh, bl = P.nb(), P.nb()
        TR([(bankb(bh)[:, cc * 32:cc * 32 + 31], sw_hi[:, cc * 128:(cc + 1) * 128]) for cc in range(4)], identb[0:31, 0:31],
           r=["junk", "identb"], w=[f"ps{bh}"])
        TR([(bankb(bl)[:, cc * 32:cc * 32 + 31], sw_lo[:, cc * 128:(cc + 1) * 128]) for cc in range(4)], identb[0:31, 0:31],
           r=["junk", "identb"], w=[f"ps{bl}"])
        MS("dve", wdwT, 0.0, w=["wdwT"])
        CP("dve", wdwT[:, :, 0:31], bankb(bh)[:, 0:128].rearrange("p (a b) -> p a b", a=4)[:, :, 0:31], r=[f"ps{bh}"], w=["wdwT"])
        TT("dve", wdwT[:, :, 0:31], wdwT[:, :, 0:31], bankb(bl)[:, 0:128].rearrange("p (a b) -> p a b", a=4)[:, :, 0:31], ALU.add,
           r=[f"ps{bl}", "wdwT"], w=["wdwT"])
        bh, bl = P.nb(), P.nb()
        TR([(bankb(bh)[:, 0:28], sv_hi)], identb[0:28, 0:28], r=["og0", "identb"], w=[f"ps{bh}"])
        TR([(bankb(bl)[:, 0:28], sv_lo)], identb[0:28, 0:28], r=["og0", "identb"], w=[f"ps{bl}"])
        CP("dve", vecs, bankb(bh)[:, 0:28], r=[f"ps{bh}"], w=["vecs"])
        TT("dve", vecs, vecs, bankb(bl)[:, 0:28], ALU.add, r=[f"ps{bl}", "vecs"], w=["vecs"])
        ACT(cT, vecs[:, 20:28], AF.Silu, r=["vecs"], w=["cT"])
        CP("dve", crep, cT.unsqueeze(2).to_broadcast([128, 8, 128]), r=["cT"], w=["crep"])

        wcnt = [0]
        zf_state = [0, 0]

        def wload(src_ap, shape3=None):
            s = wcnt[0] % NWB
            wcnt[0] += 1
            dst = wbuf[s]
            if shape3 is not None:
                dst = dst.rearrange("p a b -> p (a b)").rearrange("p (a b) -> p a b", a=shape3[0])
            DMA("pool", dst, src_ap, r=[], w=[f"wb{s}"], dsem=f"wb{s}")
            return dst, f"wb{s}"

        bb = [(lnt, "lnt"), (mean_sb, "mean_sb")]

        def ADA(j, nbf):
            src = w_ada[:, j * 512:(j + 1) * 512] if j < 12 else w_ada_f[:, (j - 12) * 512:(j - 11) * 512]
            bsrc = b_ada[:, j * 512:(j + 1) * 512] if j < 12 else b_ada_f[:, (j - 12) * 512:(j - 11) * 512]
            wb_, rwb = wload(src.rearrange("(k p) f -> p k f", p=128))
            bbuf, bname = bb[j % 2]
            DMA("sp", bbuf, bsrc.partition_broadcast(128), r=[], w=[bname], dsem=f"bb{j % 2}")
            b = nbf()
            MM(bankf(b), [(crep[:, kc, :], wb_[:, kc, :]) for kc in range(8)], r=["crep", rwb], w=[f"ps{b}"])
            m, hf = j // 2, j % 2
            TT("dve", mod(m, hf * 512, (hf + 1) * 512), bankf(b), bbuf, ALU.add,
               r=[f"ps{b}", bname], w=[f"mod{m}"])

        def GSCALE(m, gsrc):
            DMA("sp", tmpA, gsrc.partition_broadcast(128), r=[], w=["tmpA"], dsem="gld")
            STT("dve", mod(m), mod(m), 1.0, tmpA, ALU.add, ALU.mult, r=[f"mod{m}", "tmpA"], w=[f"mod{m}"])

        for j in range(4):
            ADA(j, P.nb)
        GSCALE(1, g_norm1)

        wcnt = [0]
        zf_state = [0, 0]

        def wload(src_ap, shape3=None):
            s = wcnt[0] % NWB
            wcnt[0] += 1
            dst = wbuf[s]
            if shape3 is not None:
                dst = dst.rearrange("p a b -> p (a b)").rearrange("p (a b) -> p a b", a=shape3[0])
            DMA("pool", dst, src_ap, r=[], w=[f"wb{s}"], dsem=f"wb{s}")
            return dst, f"wb{s}"

        def win(c0):
            return wload(w_in[:, c0:c0 + 512].rearrange("(k p) f -> p k f", p=128))

        def bank_cycle(lst):
            st = [0]

            def nxt():
                b = lst[st[0] % len(lst)]
                st[0] += 1
                return b
            return nxt

        def zipper(chain, heavy):
            for i in range(max(len(chain), len(heavy))):
                if i < len(chain):
                    chain[i]()
                if i < len(heavy):
                    heavy[i]()

        for q in range(NQ):
            def PA_a(t, q=q):
                gt = 4 * q + t
                xx = xr[gt % 2]
                rx_ = f"xr{gt % 2}"
                DMA("sp", xx, x[gt * 128:(gt + 1) * 128, :], r=[], w=[rx_], dsem=rx_)
                ACT(junk, xx, AF.Square, r=[rx_], w=["junk", f"ss1_{gt}"], accum_out=ss1[:, gt:gt + 1])
                RSTD(rstd1[:, gt:gt + 1], ss1[:, gt:gt + 1], 1024.0, r=[f"ss1_{gt}"], w=[f"rstd1_{gt}"])

            def PA_b(t, q=q):
                gt = 4 * q + t
                xx = xr[gt % 2]
                rx_ = f"xr{gt % 2}"
                STT("dve", tmpA, xx, rstd1[:, gt:gt + 1], mod(1), ALU.mult, ALU.mult,
                    r=[rx_, f"rstd1_{gt}", "mod1"], w=["tmpA"])
                u = ut[gt % 2]
                ru = f"ut{gt % 2}"
                TT("dve", u, tmpA, mod(0), ALU.add, r=["tmpA", "mod0"], w=[ru])
                b = P.nb()
                pT = bankb(b).rearrange("p (a b) -> p a b", a=8)
                TR([(pT[:, kc, :], u[:, kc * 128:(kc + 1) * 128]) for kc in range(8)], identb,
                   r=[ru, "identb"], w=[f"ps{b}"])
                ACT(uT_q[:, :, t * 128:(t + 1) * 128], pT, AF.Copy, r=[f"ps{b}"], w=["uT_q"])

            def B1():
                b = P.nb()
                MM(bankf(b)[0:16, :], [(wa1[:, kc, :], uT_q[:, kc, :]) for kc in range(8)], r=["wa1", "uT_q"], w=[f"ps{b}"])
                ACT(a1T[0:16, :], bankf(b)[0:16, :], AF.Copy, r=[f"ps{b}"], w=["a1T"])

            def Qp(nbf):
                wq_, rq = win(1024)
                for mc in range(4):
                    b = nbf()
                    MM(bankf(b), [(wq_[:, kc, mc * 128:(mc + 1) * 128], uT_q[:, kc, :]) for kc in range(8)], r=[rq, "uT_q"], w=[f"ps{b}"])
                    ACT(qT[:, mc, :], bankf(b), AF.Copy, r=[f"ps{b}"], w=[f"qT{mc}"], scale=float(128 ** -0.5))

            def Kp(nbf):
                wk_, rk = win(1536)
                for mc in range(4):
                    b = nbf()
                    MM(bankf(b), [(wk_[:, kc, mc * 128:(mc + 1) * 128], uT_q[:, kc, :]) for kc in range(8)], r=[rk, "uT_q"], w=[f"ps{b}"])
                    CP("dve", kT[:, mc, :], bankf(b), r=[f"ps{b}"], w=[f"kT{mc}"])

            def GATE_a(h, nbf):
                gx, gy = gX[h % 2], gY[h % 2]
                rx, ry = f"gX{h % 2}", f"gY{h % 2}"
                b = nbf()
                MM(bankf(b), [(wa2aug[:, h * 128:(h + 1) * 128], a1T)], r=["wa2aug", "a1T"], w=[f"ps{b}"])
                ACT(gx, bankf(b), AF.Exp, r=[f"ps{b}"], w=[rx], scale=-1.0)
                ACT(gx, gx, AF.Ln, r=[rx], w=[rx], bias=1.0)
                P.add("dve", (lambda gx=gx, gy=gy: (lambda e: e.tensor_tensor_scan(out=gy, data0=resetm, data1=gx, initial=0.0,
                                                                                    op0=ALU.mult, op1=ALU.add)))(), r=[rx, "resetm"], w=[ry])
                ACT(gx, gy, AF.Exp, r=[ry], w=[rx], scale=-1.0 / 16)
                ACT(gy, gy, AF.Exp, r=[ry], w=[ry], scale=1.0 / 16)

            def GATE_b(h):
                gx, gy = gX[h % 2], gY[h % 2]
                rx, ry = f"gX{h % 2}", f"gY{h % 2}"
                TT("dve", qT[:, h, :], qT[:, h, :], gx, ALU.mult, r=[f"qT{h}", rx], w=[f"qT{h}"])
                TT("dve", kT[:, h, :], kT[:, h, :], gy, ALU.mult, r=[f"kT{h}", ry], w=[f"kT{h}"])
                CP("dve", ebl[:, h, :], gx.rearrange("p (t c) -> p t c", t=4)[:, :, 127], r=[rx], w=[f"ebl{h}"])
                for t in range(4):
                    TS1("dve", klT[:, h, t * 128:(t + 1) * 128], kT[:, h, t * 128:(t + 1) * 128], ebl[:, h, t:t + 1],
                        ALU.mult, r=[f"kT{h}", f"ebl{h}"], w=[f"klT{h}"])

            def KL(nbf):
                for t in range(4):
                    b = nbf()
                    pT = bankb(b).rearrange("p (a b) -> p a b", a=8)
                    TR([(pT[:, h, :], klT[:, h, t * 128:(t + 1) * 128]) for h in range(4)], identb,
                       r=[f"klT{h}" for h in range(4)] + ["identb"], w=[f"ps{b}"])
                    ACT(kl[:, t, :, :], pT[:, 0:4, :], AF.Copy, r=[f"ps{b}"], w=[f"kl{t}"])

            def VP(hf, nbf):
                wv_, rv = win(2048 + hf * 512)
                for t in range(4):
                    b = nbf()
                    MM(bankf(b), [(uT_q[:, kc, t * 128:(t + 1) * 128], wv_[:, kc, :]) for kc in range(8)], r=[rv, "uT_q"], w=[f"ps{b}"])
                    CP("dve", v_q[:, t, hf * 512:(hf + 1) * 512], bankf(b), r=[f"ps{b}"], w=["v_q"])

            def RP(hf, nbf):
                wr_, rr = win(3072 + hf * 512)
                for t in range(4):
                    b = nbf()
                    MM(bankf(b), [(uT_q[:, kc, t * 128:(t + 1) * 128], wr_[:, kc, :]) for kc in range(8)], r=[rr, "uT_q"], w=[f"ps{b}"])
                    ACT(sr_q[:, t, hf * 512:(hf + 1) * 512], bankf(b), AF.Silu, r=[f"ps{b}"], w=["sr_q"])

            def CI(nbf):
                wa_, ra = win(0)
                wg_, rg = win(512)
                for mc in range(4):
                    ba, bg = nbf(), nbf()
                    MM(bankf(ba), [(wa_[:, kc, mc * 128:(mc + 1) * 128], uT_q[:, kc, :]) for kc in range(8)], r=[ra, "uT_q"], w=[f"ps{ba}"])
                    MM(bankf(bg), [(wg_[:, kc, mc * 128:(mc + 1) * 128], uT_q[:, kc, :]) for kc in range(8)], r=[rg, "uT_q"], w=[f"ps{bg}"])
                    ACT(sgt[mc % 2], bankf(bg), AF.Sigmoid, r=[f"ps{bg}"], w=[f"sgt{mc % 2}"])
                    TT("dve", zT[:, mc, 30:542], bankf(ba), sgt[mc % 2], ALU.mult, r=[f"ps{ba}", f"sgt{mc % 2}"], w=[f"zT{mc}"])

            def CVb(cc):
                for k in range(31):
                    TS1("dve", diag[:, k, :], identb, wdwT[:, cc, k:k + 1], ALU.mult, r=["identb", "wdwT"], w=[f"dg{k}"])

            def CVm(cc, nbf):
                b = nbf()
                for (k0, k1) in ((0, 16), (16, 31)):
                    P.add("pe", (lambda k0=k0, k1=k1, b=b, cc=cc: (lambda e: [e.matmul(bankf(b), lhsT=diag[:, k, :], rhs=zT[:, cc, k:k + 512],
                                                                                       start=(k == 0), stop=(k == 30))
                                                                              for k in range(k0, k1)][-1]))(),
                          r=[f"dg{k}" for k in range(k0, k1)] + [f"zT{cc}"], w=[f"ps{b}"])
                ACT(zc[:, cc, :], bankf(b), AF.Identity, r=[f"ps{b}", "vecs"], w=[f"zc{cc}"], bias=vecs[:, cc:cc + 1])
                ACT(zsq[:, cc, :], bankf(b), AF.Square, r=[f"ps{b}", "vecs"], w=[f"zsq{cc}"], bias=vecs[:, cc:cc + 1])
                CP("dve", zT[:, cc, 0:30], zT[:, cc, 512:542], r=[f"zT{cc}"], w=[f"zT{cc}"])

            def LN(nbf):
                bm, bq = nbf(), nbf()
                MM(bankf(bm), [(ones_bf, zc[:, cc, :]) for cc in range(4)], r=["ones_bf"] + [f"zc{cc}" for cc in range(4)], w=[f"ps{bm}"])
                MM(bankf(bq), [(ones_bf, zsq[:, cc, :]) for cc in range(4)], r=["ones_bf"] + [f"zsq{cc}" for cc in range(4)], w=[f"ps{bq}"])
                TS1("dve", mean_sb, bankf(bm), 1.0 / 512, ALU.mult, r=[f"ps{bm}"], w=["mean_sb"])
                TT("dve", lnt, mean_sb, mean_sb, ALU.mult, r=["mean_sb"], w=["lnt"])
                STT("dve", rstd_sb, bankf(bq), 1.0 / 512, lnt, ALU.mult, ALU.subtract, r=[f"ps{bq}", "lnt"], w=["rstd_sb"])
                ACT(rstd_sb, rstd_sb, AF.Ln, r=["rstd_sb"], w=["rstd_sb"], bias=EPS)
                ACT(rstd_sb, rstd_sb, AF.Exp, r=["rstd_sb"], w=["rstd_sb"], scale=-0.5)
                for cc in range(4):
                    TT("dve", lnt, zc[:, cc, :], mean_sb, ALU.subtract, r=[f"zc{cc}", "mean_sb"], w=["lnt"])
                    TT("dve", lnt, lnt, rstd_sb, ALU.mult, r=["lnt", "rstd_sb"], w=["lnt"])
                    ACT(zsq[:, cc, :], lnt, AF.Silu, r=["lnt", "vecs"], w=[f"zsq{cc}"], scale=vecs[:, 4 + cc:5 + cc], bias=vecs[:, 8 + cc:9 + cc])

            gsig = [gX[0], gX[1]]

            gs_w = {}

            def GS(col0, hf, nbf, mcs=(0, 1, 2, 3)):
                if (col0, hf) not in gs_w:
                    gs_w[(col0, hf)] = win(col0 + hf * 512)
                wg0, rg0 = gs_w[(col0, hf)]
                for mc in mcs:
                    b = nbf()
                    MM(bankf(b), [(wg0[:, kc, mc * 128:(mc + 1) * 128], uT_q[:, kc, :]) for kc in range(8)], r=[rg0, "uT_q"], w=[f"ps{b}"])
                    ACT(gsig[mc % 2], bankf(b), AF.Exp, r=[f"ps{b}"], w=[f"gX{mc % 2}"], scale=-1.0)
                    ACT(gsig[mc % 2], gsig[mc % 2], AF.Ln, r=[f"gX{mc % 2}"], w=[f"gX{mc % 2}"], bias=1.0)
                    ACT(g0s[:, hf * 4 + mc, :], gsig[mc % 2], AF.Exp, r=[f"gX{mc % 2}"], w=["g0s"], scale=-1.0)

            pw_w = {}

            def PW(nbf, dchs=range(8)):
                if "w" not in pw_w:
                    pw_w["w"] = wload(w_conv_pw.rearrange("(k p) f -> p k f", p=128), shape3=(4, 1024))
                wpw, rpw = pw_w["w"]
                for dch in dchs:
                    b = nbf()
                    MM(bankf(b), [(wpw[:, cc, dch * 128:(dch + 1) * 128], zsq[:, cc, :]) for cc in range(4)], r=[rpw] + [f"zsq{cc}" for cc in range(4)], w=[f"ps{b}"])
                    STT("dve", m0T[:, dch, :], bankf(b), vecs[:, 12 + dch:13 + dch], g0s[:, dch, :], ALU.add, ALU.mult,
                        r=[f"ps{b}", "vecs", "g0s"], w=["m0T"])

            BA, BO, BS = 0, (1, 2), (3, 4)

            def oap_(h):
                return bankf(BO[h // 2])[:, (h % 2) * 256:(h % 2 + 1) * 256]

            def REC_front(t, q=q):
                gt = 4 * q + t
                tc = slice(t * 128, (t + 1) * 128)
                for h in range(4):
                    MM(bankf(BA)[:, h * 128:(h + 1) * 128], [(kT[:, h, tc], qT[:, h, tc])], r=[f"kT{h}", f"qT{h}"], w=[f"ps{BA}"])
                TT("dve", attnT, bankf(BA).rearrange("p (a b) -> p a b", a=4), mask4, ALU.mult, r=[f"ps{BA}", "mask4"], w=["attnT"])
                for h in range(4):
                    pairs = [(attnT[:, h, :], v_q[:, t, h * 256:(h + 1) * 256])]
                    if gt > 0:
                        pairs.append((qT[:, h, tc], S16[:, h, :]))
                    MM(oap_(h), pairs, r=["attnT", "v_q", f"qT{h}", "S16"], w=[f"ps{BO[h // 2]}"])
                if gt < NT - 1:
                    for h in range(4):
                        sap = bankf(BS[h // 2])[:, (h % 2) * 256:(h % 2 + 1) * 256]
                        MM(sap, [(kl[:, t, h, :], v_q[:, t, h * 256:(h + 1) * 256])], r=[f"kl{t}", "v_q"], w=[f"ps{BS[h // 2]}"])
                    for h in range(4):
                        sap = bankf(BS[h // 2])[:, (h % 2) * 256:(h % 2 + 1) * 256]
                        if gt == 0:
                            CP("dve", S32[:, h, :], sap, r=[f"ps{BS[h // 2]}"], w=["S32"])
                        else:
                            STT("dve", S32[:, h, :], S32[:, h, :], ebl[:, h, t:t + 1], sap, ALU.mult, ALU.add,
                                r=["S32", f"ebl{h}", f"ps{BS[h // 2]}"], w=["S32"])
                    ACT(S16, S32, AF.Copy, r=["S32"], w=["S16"])

            def REC_post1(t, q=q):
                gt = 4 * q + t
                for h in range(4):
                    ACT(junk[:, 0:256], oap_(h), AF.Square, r=[f"ps{BO[h // 2]}"], w=["junk", "oss"], accum_out=oss[:, h:h + 1])
                RSTD(orstd, oss, 256.0, r=["oss"], w=["orstd"])
                TT("dve", tmpA, g_gla_bc, sr_q[:, t, :], ALU.mult, r=["g_gla_bc", "sr_q"], w=["tmpA"])
                ogt = og[gt % 2]
                for h in range(4):
                    STT("dve", ogt[:, h * 256:(h + 1) * 256], oap_(h), orstd[:, h:h + 1], tmpA[:, h * 256:(h + 1) * 256], ALU.mult, ALU.mult,
                        r=[f"ps{BO[h // 2]}", "orstd", "tmpA"], w=[f"og{gt % 2}"])

            def REC_post2(t, q=q):
                gt = 4 * q + t
                tc = slice(t * 128, (t + 1) * 128)
                ogt = og[gt % 2]
                pT = bankb(BA).rearrange("p (a b) -> p a b", a=8)
                TR([(pT[:, ec, :], ogt[:, ec * 128:(ec + 1) * 128]) for ec in range(8)], identb, r=[f"og{gt % 2}", "identb"], w=[f"ps{BA}"])
                ACT(ogT_q[:, :, tc], pT, AF.Copy, r=[f"ps{BA}"], w=["ogT_q"])

            def GO(nbf):
                for hf in range(2):
                    wgo, rgo = wload(w_gla_o[:, hf * 512:(hf + 1) * 512].rearrange("(k p) f -> p k f", p=128))
                    for mc in range(4):
                        dch = hf * 4 + mc
                        b = nbf()
                        MM(bankf(b), [(wgo[:, ec, mc * 128:(mc + 1) * 128], ogT_q[:, ec, :]) for ec in range(8)], r=[rgo, "ogT_q"], w=[f"ps{b}"])
                        TT("dve", tmpA[:, 0:512], bankf(b), g0s[:, dch, :], ALU.mult, r=[f"ps{b}", "g0s"], w=["tmpA"])
                        TT("dve", m0T[:, dch, :], tmpA[:, 0:512], m0T[:, dch, :], ALU.add, r=["tmpA", "m0T"], w=["m0T"])

            wo = [None, None]

            ob = {}

            def OUT_mm(t, q=q):
                ob[t] = (P.nb(), P.nb())
                for hf in range(2):
                    b = ob[t][hf]
                    MM(bankf(b), [(m0T[:, kc, t * 128:(t + 1) * 128], wo[hf][0][:, kc, :]) for kc in range(8)], r=["m0T", wo[hf][1]], w=[f"ps{b}"])

            def OUT_res(t, q=q):
                gt = 4 * q + t
                xx = xr[gt % 2]
                rx_ = f"xr{gt % 2}"
                DMA("sp", xx, x[gt * 128:(gt + 1) * 128, :], r=[], w=[rx_], dsem=rx_)
                for hf in range(2):
                    b = ob[t][hf]
                    TT("dve", tmpA[:, hf * 512:(hf + 1) * 512], bankf(b), mod(2, hf * 512, (hf + 1) * 512), ALU.mult, r=[f"ps{b}", "mod2"], w=["tmpA"])
                    TT("dve", xx[:, hf * 512:(hf + 1) * 512], tmpA[:, hf * 512:(hf + 1) * 512], xx[:, hf * 512:(hf + 1) * 512], ALU.add,
                       r=["tmpA", rx_], w=[rx_])
                DMA("sp", h_scr[gt * 128:(gt + 1) * 128, :], xx, r=[rx_], w=[f"hscr{gt}"], dsem=f"hout{gt % 2}")
                ACT(junk, xx, AF.Square, r=[rx_], w=["junk", f"ss2_{gt}"], accum_out=ss2[:, gt:gt + 1])
                RSTD(rstd2[:, gt:gt + 1], ss2[:, gt:gt + 1], 1024.0, r=[f"ss2_{gt}"], w=[f"rstd2_{gt}"])

            lgb = {}

            def OUT_back(t, q=q):
                gt = 4 * q + t
                xx = xr[gt % 2]
                rx_ = f"xr{gt % 2}"
                STT("dve", tmpA, xx, rstd2[:, gt:gt + 1], mod(4), ALU.mult, ALU.mult,
                    r=[rx_, f"rstd2_{gt}", "mod4"], w=["tmpA"])
                u2hi = ut[gt % 2]
                ru = f"ut{gt % 2}"
                TT("dve", u2hi, tmpA, mod(3), ALU.add, r=["tmpA", "mod3"], w=[ru])
                b = P.nb()
                pT = bankb(b).rearrange("p (a b) -> p a b", a=8)
                TR([(pT[:, kc, :], u2hi[:, kc * 128:(kc + 1) * 128]) for kc in range(8)], identb, r=[ru, "identb"], w=[f"ps{b}"])
                u2t = og[gt % 2].rearrange("p (a b) -> p a b", a=8)
                ACT(u2t, pT, AF.Copy, r=[f"ps{b}"], w=[f"og{gt % 2}"])
                DMA("sp", u2_scr[gt * 128:(gt + 1) * 128, :], u2hi, r=[ru], w=[f"u2scr{gt}"], dsem=f"u2out{gt % 2}")
                b = P.nb()
                lgb[t] = b
                MM(bankf(b)[:, 0:36], [(u2t[:, kc, :], wr_bf[:, kc, :]) for kc in range(8)], r=[f"og{gt % 2}", "wr_bf"], w=[f"ps{b}"])

            def OUT_la(t, q=q):
                gt = 4 * q + t
                b = lgb[t]
                TT("dve", logits[:, gt, :], bankf(b)[:, 0:36], br_bc, ALU.add, r=[f"ps{b}", "br_bc"], w=["logits"])

            PA_a(0)
            PA_a(1)
            for t in range(4):
                PA_b(t)
                if t + 2 < 4:
                    PA_a(t + 2)
            B1()
            nb1 = bank_cycle([2, 3, 4, 5, 6, 7])
            nbc = bank_cycle([0, 1])
            zipper([lambda: GATE_a(0, nbc), lambda: GATE_a(1, nbc), lambda: (GATE_b(0), GATE_a(2, nbc)), lambda: (GATE_b(1), GATE_a(3, nbc)),
                    lambda: GATE_b(2), lambda: (GATE_b(3), KL(nbc))],
                   [lambda: Qp(nb1), lambda: Kp(nb1), lambda: VP(0, nb1), lambda: VP(1, nb1)]
                   + ([lambda: ADA(4 + 6 * q, nb1), lambda: ADA(5 + 6 * q, nb1)] if q < 2 else []))
            CVb(0)
            CI(P.nb)
            for cc in range(4):
                CVm(cc, P.nb)
                if cc + 1 < 4:
                    CVb(cc + 1)
            LN(P.nb)
            RP(0, P.nb)
            RP(1, P.nb)
            nb2 = bank_cycle([5, 6, 7])
            zf = [0]

            def ZF(n, q=q):
                if q == 0 and zf[0] == 0:
                    MS("dve", ztile, 0.0, w=["ztile"])
                for _ in range(n):
                    r0 = zf_state[0]
                    if r0 >= NSLOT:
                        return
                    na = min(8, (NSLOT - r0) // 128)
                    hf = zf_state[1]
                    DMA("sp", X_scr[r0:r0 + na * 128, hf * 512:(hf + 1) * 512].rearrange("(a p) d -> p a d", p=128),
                        ztile.unsqueeze(1).to_broadcast([128, na, 512]), r=["ztile"], w=[f"Xz{r0}_{hf}"], dsem="xz")
                    zf[0] += 1
                    if hf == 1:
                        zf_state[0] = r0 + na * 128
                    zf_state[1] = 1 - hf

            def ADAq(i, q=q):
                if q < 2:
                    ADA(6 + 6 * q + i, nb2)

            HA = [lambda: GS(4112, 0, nb2, (0, 1)), lambda: GS(4112, 1, nb2, (0, 1)), lambda: PW(nb2, range(0, 4)),
                  lambda: GS(5136, 0, nb2, (0, 1)), lambda: GS(5136, 1, nb2, (0, 1))]
            HB = [lambda: GS(4112, 0, nb2, (2, 3)), lambda: GS(4112, 1, nb2, (2, 3)), lambda: PW(nb2, range(4, 8)),
                  lambda: GS(5136, 0, nb2, (2, 3)), lambda: GS(5136, 1, nb2, (2, 3))]
            for i in range(5):
                if i < 4:
                    REC_front(i)
                if i >= 1:
                    REC_post2(i - 1)
                HA[i]()
                if i < 4:
                    REC_post1(i)
                HB[i]()
                ZF(2 if (i < 4 or q < 2) else 100)
                if i < 4:
                    ADAq(i)
            GO(P.nb)
            if q == 0:
                GSCALE(4, g_norm2)
            elif q == 1:
                GSCALE(7, g_final)
            wo[0] = wload(w_out[:, 0:512].rearrange("(k p) f -> p k f", p=128))
            wo[1] = wload(w_out[:, 512:1024].rearrange("(k p) f -> p k f", p=128))
            OUT_mm(0)
            OUT_res(0)
            OUT_mm(1)
            OUT_res(1)
            for t in range(4):
                if t + 2 < 4:
                    OUT_mm(t + 2)
                OUT_back(t)
                if t + 2 < 4:
                    OUT_res(t + 2)
                OUT_la(t)

        DBG("logits", logits.rearrange("p a b -> p (a b)"), [128, NT * 36], r=["logits"])

        P.barrier()
        A.release(phase_mark)
        slots_i = es.enter_context(nc.sbuf_tensor("slots_i", [128, NT * 2], I32))[:, :].rearrange("p (a b) -> p a b", a=NT)
        wt1 = A.alloc([128, NT], F32)
        wt2 = A.alloc([128, NT], F32)
        widx_i = es.enter_context(nc.sbuf_tensor("widx_i", [128, NOT], I32))[:, :]
        moe_tmp_mark = A.mark()
        rb_slot = es.enter_context(nc.gpsimd.register("rb_slot"))
        rb_w = es.enter_context(nc.gpsimd.register("rb_w"))
        P.add("pool", lambda e: (e.reg_mov(rb_slot, NSLOT - 1), e.reg_mov(rb_w, NE * 128 - 1))[-1])
        NWS = 3
        wS = [A.alloc([128, 12288], BF16) for _ in range(NWS + 2)]

        def wslot(ws):
            return ws % NWS if ws < NE else NWS + (ws - NE) % 2
        wE = [(w_[:, 0:4096].rearrange("p (a b) -> p a b", a=8), w_[:, 4096:8192].rearrange("p (a b) -> p a b", a=8),
               w_[:, 8192:12288].rearrange("p (a b) -> p a b", a=4)) for w_ in wS]
        route_mark = A.mark()
        u2tm = A.alloc([128, NT, 1024], BF16)
        Lst = A.alloc([128, 128], BF16)
        gmax = A.alloc([128, NT], F32)
        gmask = A.alloc([128, NT, 4], F32)
        eg = A.alloc([128, NT, 4], F32)
        gsum = A.alloc([128, NT], F32)
        wgt = A.alloc([128, NT], F32)
        lem = A.alloc([128, NT, NE], F32)
        pen = A.alloc([128, NT, NE], F32)
        mk1 = A.alloc([128, NT, NE], F32)
        mk2 = A.alloc([128, NT, NE], F32)
        m1 = A.alloc([128, NT], F32)
        m2 = A.alloc([128, NT], F32)
        dd = A.alloc([128, NT], F32)
        Mb = A.alloc([128, NT * NE], BF16)
        cs_ei = A.alloc([128, NE, NT], F32)
        resetE = A.alloc([128, NE, NT], F32)
        incl = A.alloc([128, NE, NT], F32)
        excl = A.alloc([128, NE, NT], F32)
        rank = A.alloc([128, NT, NE], F32)
        iota_i = es.enter_context(nc.sbuf_tensor("iota_i", [128, 32], I32))[:, :]
        iota_f = A.alloc([128, 32], F32)
        ones32 = A.alloc([128, 32], F32)
        o_e = A.alloc([128, NE], F32)
        thr14 = A.alloc([128, 14], F32)
        cmp14 = A.alloc([128, NE, 14], F32)
        ot_e = A.alloc([128, NE], F32)
        osz = A.alloc([128, NE], F32)
        oincl = A.alloc([128, NE], F32)
        obase = A.alloc([128, NE], F32)
        thrJ = A.alloc([128, NOT], F32)
        cmpJ = A.alloc([128, NOT, NE], F32)
        oe_f = A.alloc([128, NOT], F32)
        iotap_i = es.enter_context(nc.sbuf_tensor("iotap_i", [128, 1], I32))[:, :]
        iotap_f = A.alloc([128, 1], F32)
        eC = A.alloc([128, NE], F32)
        Bv = A.alloc([128, NE], F32)
        isov = A.alloc([128, NT, NE], F32)
        sval = A.alloc([128, NT, NE], F32)
        slots_f = A.alloc([128, NT, 2], F32)
        print("arena words used (moe phase):", A.mark())

        loaded = set()

        def load_weights(ws):
            if ws in loaded:
                return
            loaded.add(ws)
            s_ = wslot(ws)
            if ws < NE:
                DMA("pool", wS[s_], wpack[ws * 128:(ws + 1) * 128, :], r=[], w=[f"wE{s_}"], dsem=f"wE{s_}")
            else:
                j = ws - NE
                P.add("pool", (lambda j=j, s_=s_: (lambda e: e.indirect_dma_start(
                    out=wS[s_], out_offset=None, in_=wpack[:, :],
                    in_offset=bass.IndirectOffsetOnAxis(ap=widx_i[:, j:j + 1], axis=0),
                    bounds_check=rb_w, oob_is_err=False)))(), r=["widx_i"], w=[f"wE{s_}"], dsem=f"wE{s_}")

        load_weights(0)
        DMA("sp", u2tm, u2_scr.rearrange("(i p) d -> p i d", p=128), r=[], w=["u2tm"], dsem="u2in")
        MS("pool", Lst, 1.0, w=["Lst"])
        P.add("pool", lambda e: e.affine_select(out=Lst, in_=Lst, pattern=[[1, 128]], compare_op=ALU.is_ge,
                                                fill=0.0, base=-1, channel_multiplier=-1), r=["Lst"], w=["Lst"])
        P.add("pool", lambda e: e.iota(out=iota_i, pattern=[[1, 32]], base=0, channel_multiplier=0), w=["iota_i"])
        CP("dve", iota_f, iota_i, r=["iota_i"], w=["iota_f"])
        MS("dve", ones32, 1.0, w=["ones32"])
        MS("dve", resetE, 1.0, w=["resetE"])
        MS("dve", resetE[:, :, 0:1], 0.0, w=["resetE"])

        lg = logits[:, :, 0:4]
        le = logits[:, :, 4:36]
        P.add("dve", lambda e: e.tensor_reduce(out=gmax, in_=lg, axis=AX.X, op=ALU.max), r=["logits"], w=["gmax"])
        TT("dve", gmask, lg, gmax.unsqueeze(2).to_broadcast([128, NT, 4]), ALU.is_equal, r=["logits", "gmax"], w=["gmask"])
        TT("dve", eg, lg, gmax.unsqueeze(2).to_broadcast([128, NT, 4]), ALU.subtract, r=["logits", "gmax"], w=["eg"])
        ACT(eg, eg, AF.Exp, r=["eg"], w=["eg"])
        P.add("dve", lambda e: e.tensor_reduce(out=gsum, in_=eg, axis=AX.X, op=ALU.add), r=["eg"], w=["gsum"])
        P.add("dve", lambda e: e.reciprocal(out=wgt, in_=gsum), r=["gsum"], w=["wgt"])
        CP("dve", pen.rearrange("p t (g e) -> p t g e", g=4), gmask.unsqueeze(3).to_broadcast([128, NT, 4, 8]), r=["gmask"], w=["pen"])
        TS("dve", pen, pen, 1e30, -1e30, ALU.mult, ALU.add, r=["pen"], w=["pen"])
        TT("dve", lem, le, pen, ALU.add, r=["logits", "pen"], w=["lem"])
        P.add("dve", lambda e: e.tensor_reduce(out=m1, in_=lem, axis=AX.X, op=ALU.max), r=["lem"], w=["m1"])
        TT("dve", mk1, lem, m1.unsqueeze(2).to_broadcast([128, NT, NE]), ALU.is_equal, r=["lem", "m1"], w=["mk1"])
        STT("dve", lem, mk1, -1e30, lem, ALU.mult, ALU.add, r=["mk1", "lem"], w=["lem"])
        P.add("dve", lambda e: e.tensor_reduce(out=m2, in_=lem, axis=AX.X, op=ALU.max), r=["lem"], w=["m2"])
        TT("dve", mk2, lem, m2.unsqueeze(2).to_broadcast([128, NT, NE]), ALU.is_equal, r=["lem", "m2"], w=["mk2"])
        TT("dve", dd, m2, m1, ALU.subtract, r=["m1", "m2"], w=["dd"])
        ACT(dd, dd, AF.Exp, r=["dd"], w=["dd"])
        TS1("dve", dd, dd, 1.0, ALU.add, r=["dd"], w=["dd"])
        P.add("dve", lambda e: e.reciprocal(out=dd, in_=dd), r=["dd"], w=["dd"])
        TT("dve", wt1, wgt, dd, ALU.mult, r=["wgt", "dd"], w=["wt1"])
        TT("dve", wt2, wgt, wt1, ALU.subtract, r=["wgt", "wt1"], w=["wt2"])

        TT("dve", Mb.rearrange("p (t e) -> p t e", t=NT), mk1, mk2, ALU.add, r=["mk1", "mk2"], w=["Mb"])
        bR1, bR2 = P.nb(), P.nb()
        MM(bankf(bR1), [(Lst, Mb)], r=["Lst", "Mb"], w=[f"ps{bR1}"])
        MM(bankf(bR2), [(ones_bf, Mb)], r=["ones_bf", "Mb"], w=[f"ps{bR2}"])
        CP("dve", cs_ei, bankf(bR2).rearrange("p (i e) -> p e i", i=NT), r=[f"ps{bR2}"], w=["cs_ei"])
        P.add("dve", lambda e: e.tensor_tensor_scan(out=incl.rearrange("p e i -> p (e i)"), data0=resetE.rearrange("p e i -> p (e i)"),
                                                    data1=cs_ei.rearrange("p e i -> p (e i)"), initial=0.0, op0=ALU.mult, op1=ALU.add),
              r=["resetE", "cs_ei"], w=["incl"])
        TT("dve", excl, incl, cs_ei, ALU.subtract, r=["incl", "cs_ei"], w=["excl"])
        TT("dve", rank, bankf(bR1).rearrange("p (i e) -> p i e", i=NT), excl.rearrange("p e i -> p i e"), ALU.add,
           r=[f"ps{bR1}", "excl"], w=["rank"])
        n_e = incl[:, :, NT - 1]
        TS("dve", o_e, n_e, -float(CAP), 0.0, ALU.add, ALU.max, r=["incl"], w=["o_e"])
        TS1("dve", thr14, iota_f[:, 0:14], 128.0, ALU.mult, r=["iota_f"], w=["thr14"])
        TT("dve", cmp14, o_e.unsqueeze(2).to_broadcast([128, NE, 14]), thr14.unsqueeze(1).to_broadcast([128, NE, 14]), ALU.is_gt,
           r=["o_e", "thr14"], w=["cmp14"])
        P.add("dve", lambda e: e.tensor_reduce(out=ot_e, in_=cmp14, axis=AX.X, op=ALU.add), r=["cmp14"], w=["ot_e"])
        TS1("dve", osz, ot_e, 128.0, ALU.mult, r=["ot_e"], w=["osz"])
        P.add("dve", lambda e: e.tensor_tensor_scan(out=oincl, data0=ones32, data1=osz, initial=0.0, op0=ALU.mult, op1=ALU.add),
              r=["ones32", "osz"], w=["oincl"])
        TT("dve", obase, oincl, osz, ALU.subtract, r=["oincl", "osz"], w=["obase"])
        TS1("dve", thrJ, iota_f[:, 0:NOT], 128.0, ALU.mult, r=["iota_f"], w=["thrJ"])
        TT("dve", cmpJ, oincl.unsqueeze(1).to_broadcast([128, NOT, NE]), thrJ.unsqueeze(2).to_broadcast([128, NOT, NE]), ALU.is_le,
           r=["oincl", "thrJ"], w=["cmpJ"])
        P.add("dve", lambda e: e.tensor_reduce(out=oe_f, in_=cmpJ, axis=AX.X, op=ALU.add), r=["cmpJ"], w=["oe_f"])
        P.add("pool", lambda e: e.iota(out=iotap_i, pattern=[[0, 1]], base=0, channel_multiplier=1), w=["iotap_i"])
        CP("dve", iotap_f, iotap_i, r=["iotap_i"], w=["iotap_f"])
        TS("dve", cmpJ[:, 0, 0:NOT], oe_f, 128.0, iotap_f[:, 0:1], ALU.mult, ALU.add, r=["oe_f", "cmpJ", "iotap_f"], w=["cmpJ"])
        CP("dve", widx_i, cmpJ[:, 0, 0:NOT], r=["cmpJ"], w=["widx_i"])
        TS1("dve", eC, iota_f, float(CAP), ALU.mult, r=["iota_f"], w=["eC"])
        TT("dve", Bv, obase, eC, ALU.subtract, r=["obase", "eC"], w=["Bv"])
        TS1("dve", Bv, Bv, float(NE * CAP - CAP), ALU.add, r=["Bv"], w=["Bv"])
        TS1("dve", isov, rank, float(CAP), ALU.is_ge, r=["rank"], w=["isov"])
        TT("dve", isov, isov, Bv.unsqueeze(1).to_broadcast([128, NT, NE]), ALU.mult, r=["isov", "Bv"], w=["isov"])
        TT("dve", sval, rank, eC.unsqueeze(1).to_broadcast([128, NT, NE]), ALU.add, r=["rank", "eC"], w=["sval"])
        TT("dve", sval, sval, isov, ALU.add, r=["sval", "isov"], w=["sval"])
        TT("dve", isov, mk1, sval, ALU.mult, r=["mk1", "sval"], w=["isov"])
        P.add("dve", lambda e: e.tensor_reduce(out=slots_f[:, :, 0], in_=isov, axis=AX.X, op=ALU.add), r=["isov"], w=["slots_f"])
        TT("dve", isov, mk2, sval, ALU.mult, r=["mk2", "sval", "slots_f"], w=["isov"])
        P.add("dve", lambda e: e.tensor_reduce(out=slots_f[:, :, 1], in_=isov, axis=AX.X, op=ALU.add), r=["isov"], w=["slots_f"])
        CP("dve", slots_i, slots_f, r=["slots_f"], w=["slots_i"])
        DBG("slots", slots_f.rearrange("p a b -> p (a b)"), [128, NT * 2], r=["slots_f"])
        DBG("oe", oe_f, [128, NOT], r=["oe_f"])
        DBG("wt1", wt1, [128, NT], r=["wt1"])
        DBG("wt2", wt2, [128, NT], r=["wt2"])

        scat_names = []
        for i in range(NT):
            for k in range(2):
                nm = f"Xs{i}_{k}"
                scat_names.append(nm)
                P.add("pool", (lambda i=i, k=k: (lambda e: e.indirect_dma_start(
                    out=X_scr[:, :], out_offset=bass.IndirectOffsetOnAxis(ap=slots_i[:, i, k:k + 1], axis=0),
                    in_=u2tm[:, i, :], in_offset=None, bounds_check=rb_slot, oob_is_err=False)))(),
                    r=["slots_i", "u2tm"], w=[nm], dsem="scat")

        load_weights(NE + 0)
        for ws0 in range(1, NWS):
            load_weights(ws0)

        NXB, NYB, LA = 6, 4, 5
        tiles = []
        jo = 0
        for ex in range(NE):
            for ti in range(CAP // 128):
                tiles.append((ex, ex * CAP + ti * 128))
            if ex >= 1 and jo < NOT:
                tiles.append((NE + jo, NE * CAP + jo * 128))
                jo += 1
        while jo < NOT:
            tiles.append((NE + jo, NE * CAP + jo * 128))
            jo += 1
        NTL = len(tiles)
        P.barrier(keep=("wE1", "wE2", "wE3"))
        A.release(route_mark)
        Xt = [A.alloc([128, 1024], BF16) for _ in range(NXB)]
        xT = [A.alloc([128, 8, 128], BF16) for _ in range(NXB)]
        stmp = [A.alloc([128, 512], BF16) for _ in range(2)]
        hid = [A.alloc([128, 512], BF16) for _ in range(2)]
        hidT = [A.alloc([128, 4, 128], BF16) for _ in range(2)]
        Yt = [A.alloc([128, 1024], F32) for _ in range(NYB)]
        scat_names = []

        def stageA1(n):
            ws, row = tiles[n]
            b3 = n % NXB
            DMA("sp", Xt[b3], X_scr[row:row + 128, :], r=scat_names, w=[f"Xt{b3}"], dsem=f"Xt{b3}")

        def stageA(n):
            b3 = n % NXB
            b = P.nb()
            pT = bankb(b).rearrange("p (a b) -> p a b", a=8)
            TR([(pT[:, kc, :], Xt[b3][:, kc * 128:(kc + 1) * 128]) for kc in range(8)], identb, r=[f"Xt{b3}", "identb"], w=[f"ps{b}"])
            ACT(xT[b3], pT, AF.Copy, r=[f"ps{b}"], w=[f"xT{b3}"])

        def stageB(n):
            ws, row = tiles[n]
            load_weights(ws)
            s_ = wslot(ws)
            W1, W3, W2 = wE[s_]
            b3, b2 = n % NXB, n % 2
            b1_, b3_ = P.nb(), P.nb()
            MM(bankf(b1_), [(xT[b3][:, kc, :], W1[:, kc, :]) for kc in range(8)], r=[f"xT{b3}", f"wE{s_}"], w=[f"ps{b1_}"])
            MM(bankf(b3_), [(xT[b3][:, kc, :], W3[:, kc, :]) for kc in range(8)], r=[f"xT{b3}", f"wE{s_}"], w=[f"ps{b3_}"])
            ACT(stmp[b2], bankf(b1_), AF.Silu, r=[f"ps{b1_}"], w=[f"stmp{b2}"])
            TT("dve", hid[b2], bankf(b3_), stmp[b2], ALU.mult, r=[f"ps{b3_}", f"stmp{b2}"], w=[f"hid{b2}"])

        def stageC(n):
            b2 = n % 2
            b = P.nb()
            pT = bankb(b)[:, 0:512].rearrange("p (a b) -> p a b", a=4)
            TR([(pT[:, fc, :], hid[b2][:, fc * 128:(fc + 1) * 128]) for fc in range(4)], identb, r=[f"hid{b2}", "identb"], w=[f"ps{b}"])
            CP("dve", hidT[b2], pT, r=[f"ps{b}"], w=[f"hidT{b2}"])

        def stageD(n):
            ws, row = tiles[n]
            s_ = wslot(ws)
            W2 = wE[s_][2]
            b2 = n % 2
            by = n % NYB
            for hf in range(2):
                b = P.nb()
                MM(bankf(b), [(hidT[b2][:, fc, :], W2[:, fc, hf * 512:(hf + 1) * 512]) for fc in range(4)],
                   r=[f"hidT{b2}", f"wE{s_}"], w=[f"ps{b}"])
                TT("dve", Yt[by][:, hf * 512:(hf + 1) * 512], bankf(b), mod(5, hf * 512, (hf + 1) * 512), ALU.mult,
                   r=[f"ps{b}"], w=[f"Yt{by}"])
            DMA("sp", Y_scr[row:row + 128, :], Yt[by], r=[f"Yt{by}"], w=[f"Yw{n}"], dsem=f"yout{by}")

        load_weights(NE + 0)
        for n0 in range(LA):
            stageA1(n0)
        stageA(0)
        stageA(1)
        stageB(0)
        for n in range(NTL):
            stageC(n)
            if n + 1 < NTL:
                stageB(n + 1)
            if n + LA < NTL:
                stageA1(n + LA)
            if n + 2 < NTL:
                stageA(n + 2)
            stageD(n)
            if n + 1 < NTL:
                ws1 = tiles[n + 1][0]
                if ws1 < NE and tiles[n][0] != ws1:
                    if 1 <= ws1 < NOT:
                        load_weights(NE + ws1)
                    if ws1 + 2 < NE:
                        load_weights(ws1 + 2)

        P.barrier()
        A.release(moe_tmp_mark)
        NCB = 3
        hf_t = [A.alloc([128, 1024], F32) for _ in range(NCB)]
        o_t = [A.alloc([128, 1024], F32) for _ in range(2)]
        g1_t = [A.alloc([128, 1024], F32) for _ in range(NCB)]
        g2_t = [A.alloc([128, 1024], F32) for _ in range(NCB)]
        tmpB = [A.alloc([128, 1024], F32) for _ in range(2)]
        junk2 = A.alloc([128, 1024], BF16)
        def HLOAD(gt):
            p3 = gt % NCB
            DMA("sp", hf_t[p3], h_scr[gt * 128:(gt + 1) * 128, :], r=[], w=[f"hf{p3}"], dsem=f"hf{p3}")

        for gt in range(NCB):
            HLOAD(gt)
        for gt in range(NT):
            p2 = gt % 2
            p3 = gt % NCB
            hh = hf_t[p3]
            oo = o_t[p2]
            tb = tmpB[p2]
            for (k, gbuf, nm) in ((0, g1_t[p3], f"g1_{p3}"), (1, g2_t[p3], f"g2_{p3}")):
                P.add("pool", (lambda gt=gt, k=k, gbuf=gbuf: (lambda e: e.indirect_dma_start(
                    out=gbuf, out_offset=None, in_=Y_scr[:, :],
                    in_offset=bass.IndirectOffsetOnAxis(ap=slots_i[:, gt, k:k + 1], axis=0),
                    bounds_check=rb_slot, oob_is_err=False)))(), r=[], w=[nm], dsem=nm)
            STT("dve", hh, g1_t[p3], wt1[:, gt:gt + 1], hh, ALU.mult, ALU.add, r=[f"g1_{p3}", f"hf{p3}"], w=[f"hf{p3}"])
            STT("dve", hh, g2_t[p3], wt2[:, gt:gt + 1], hh, ALU.mult, ALU.add, r=[f"g2_{p3}", f"hf{p3}"], w=[f"hf{p3}"])
            ACT(junk2, hh, AF.Square, r=[f"hf{p3}"], w=["junk2", f"ss3_{gt}"], accum_out=ss3[:, gt:gt + 1])
            RSTD(rstd3[:, gt:gt + 1], ss3[:, gt:gt + 1], 1024.0, r=[f"ss3_{gt}"], w=[f"rstd3_{gt}"])
            STT("dve", tb, hh, rstd3[:, gt:gt + 1], mod(7), ALU.mult, ALU.mult, r=[f"hf{p3}", f"rstd3_{gt}"], w=[f"tmpB{p2}"])
            TT("dve", oo, tb, mod(6), ALU.add, r=[f"tmpB{p2}"], w=[f"oo{p2}"])
            DMA("sp", out[gt * 128:(gt + 1) * 128, :], oo, r=[f"oo{p2}"], w=[f"out{gt}"], dsem=f"out{p2}")
            if gt + NCB < NT:
                HLOAD(gt + NCB)
        P.final.append(("sp", "out0"))
        P.final.append(("sp", "out1"))
        if "dbg" in P.dsems:
            P.final.append(("pool", "dbg"))
        for nm_ in ("hout0", "hout1", "u2out0", "u2out1"):
            P.final.append(("sp", nm_))

        block = es.enter_context(nc.Block())
        P.emit(block)
    return nc, dbg_outs


def pack_experts(w1, w3, w2):
    w1 = np.asarray(w1, dtype=np.float32).reshape(NE, 8, 128, 512).transpose(0, 2, 1, 3).reshape(NE, 128, 4096)
    w3 = np.asarray(w3, dtype=np.float32).reshape(NE, 8, 128, 512).transpose(0, 2, 1, 3).reshape(NE, 128, 4096)
    w2 = np.asarray(w2, dtype=np.float32).reshape(NE, 4, 128, 1024).transpose(0, 2, 1, 3).reshape(NE, 128, 4096)
    return np.ascontiguousarray(np.concatenate([w1, w3, w2], axis=2).reshape(NE * 128, 12288))


def make_in_map(inputs, b):
    f = lambda a: np.ascontiguousarray(np.asarray(a, dtype=np.float32))
    i = inputs
    return {
        "x": f(i["x"][b]),
        "c": f(i["c"][b].reshape(8, 128)),
        "w_ada": f(i["w_ada"][0]),
        "b_ada": f(i["b_ada"][0].reshape(1, 6144)),
        "g_norm1": f(i["g_norm1"][0].reshape(1, 1024)),
        "w_in": f(i["w_in"][0]),
        "w_dw": f(i["w_dw"][0].reshape(31, 512)),
        "b_dw": f(i["b_dw"][0].reshape(4, 128)),
        "g_conv_ln": f(i["g_conv_ln"][0].reshape(4, 128)),
        "b_conv_ln": f(i["b_conv_ln"][0].reshape(4, 128)),
        "w_conv_pw": f(i["w_conv_pw"][0]),
        "b_conv_pw": f(i["b_conv_pw"][0].reshape(8, 128)),
        "w_a2": f(i["w_a2"][0]),
        "b_a2": f(i["b_a2"][0].reshape(1, 512)),
        "g_gla_norm": f(i["g_gla_norm"][0].reshape(1, 1024)),
        "w_gla_o": f(i["w_gla_o"][0]),
        "w_out": f(i["w_out"][0]),
        "g_norm2": f(i["g_norm2"][0].reshape(1, 1024)),
        "w_router_g": f(i["w_router_g"][0]),
        "b_router_g": f(i["b_router_g"][0].reshape(1, 4)),
        "w_router_e": f(i["w_router_e"][0]),
        "b_router_e": f(i["b_router_e"][0].reshape(1, 32)),
        "wpack": pack_experts(i["w1"][0], i["w3"][0], i["w2"][0]),
        "w_ada_f": f(i["w_ada_f"]),
        "b_ada_f": f(i["b_ada_f"].reshape(1, 2048)),
        "g_final": f(i["g_final"].reshape(1, 1024)),
    }


_NC_CACHE = {}


def kernel(**inputs):
    if "nc" not in _NC_CACHE:
        _NC_CACHE["nc"] = build_program()[0]
    nc = _NC_CACHE["nc"]
    shared = make_in_map(inputs, 0)
    in_maps = []
    for b in range(8):
        m = dict(shared)
        m["x"] = np.ascontiguousarray(np.asarray(inputs["x"][b], dtype=np.float32))
        m["c"] = np.ascontiguousarray(np.asarray(inputs["c"][b], dtype=np.float32).reshape(8, 128))
        in_maps.append(m)
    res = run_bass_kernel_spmd(nc, in_maps, core_ids=list(range(8)))
    return np.stack([np.asarray(r["out"], dtype=np.float32) for r in res.results], axis=0)
```

```python
import numpy as np
from contextlib import ExitStack
import concourse.bass as bass
import concourse.mybir as mybir
from concourse.bass_utils import run_bass_kernel_spmd

F32 = mybir.dt.float32
BF16 = mybir.dt.bfloat16
AF = mybir.ActivationFunctionType
ALU = mybir.AluOpType
AX = mybir.AxisListType

EPS = 1e-6
NT = 16
NQ = 4
NE = 32
CAP = 256
NOT = 30
NSLOT = NE * CAP + NOT * 128
I32 = mybir.dt.int32


class Op:
    __slots__ = ("eng", "fn", "deps", "sig", "sigval", "dsem", "dval")

    def __init__(self, eng, fn):
        self.eng = eng
        self.fn = fn
        self.deps = []
        self.sig = False
        self.sigval = 0
        self.dsem = None
        self.dval = 0


class Prog:
    ENG = ("pe", "act", "dve", "pool", "sp")

    def __init__(self, nc, es):
        self.nc = nc
        self.es = es
        self.ops = {e: [] for e in self.ENG}
        self.res = {}
        self.dsems = {}
        self.esem = {}
        for e in self.ENG:
            self.esem[e] = es.enter_context(nc.semaphore("es_" + e))
        self.final = []
        self.bank = 0

    def nb(self):
        b = self.bank
        self.bank = (self.bank + 1) % 8
        return b

    def add(self, eng, fn, r=(), w=(), dsem=None):
        o = Op(eng, fn)
        deps = []
        for x in r:
            st = self.res.get(x)
            if st is not None and st[0] is not None:
                deps.append(st[0])
        for x in w:
            st = self.res.get(x)
            if st is not None:
                if st[0] is not None:
                    deps.append(st[0])
                deps.extend(st[1])
        for x in r:
            st = self.res.get(x)
            if st is None:
                st = [None, []]
                self.res[x] = st
            st[1].append(o)
        for x in w:
            self.res[x] = [o, []]
        seen = set()
        for d in deps:
            if d is o or id(d) in seen:
                continue
            seen.add(id(d))
            if d.eng == "pe" and eng == "pe" and d.dsem is None:
                continue
            o.deps.append(d)
            d.sig = True
        if dsem is not None:
            if dsem not in self.dsems:
                self.dsems[dsem] = [self.es.enter_context(self.nc.semaphore("ds_" + dsem)), 0, None]
            ent = self.dsems[dsem]
            ent[1] += 16
            ent[2] = o
            o.dsem = dsem
            o.dval = ent[1]
        self.ops[eng].append(o)
        return o

    def barrier(self, keep=()):
        lasts = []
        for e in self.ENG:
            for o in reversed(self.ops[e]):
                if o.dsem is None and o.fn is not None:
                    lasts.append(o)
                    break
        for name, ent in self.dsems.items():
            if ent[2] is not None and name not in keep:
                lasts.append(ent[2])
        for o in lasts:
            o.sig = True
        for e in self.ENG:
            b = Op(e, None)
            b.deps = list(lasts)
            self.ops[e].append(b)
        self.res = {k: v for k, v in self.res.items() if k in keep}

    def emit(self, block):
        for e in self.ENG:
            c = 0
            for o in self.ops[e]:
                if o.sig and o.dsem is None and o.fn is not None:
                    c += 1
                    o.sigval = c
        me = self

        def run(engname, eng):
            waited = {}
            for o in me.ops[engname]:
                for d in o.deps:
                    if d.dsem is not None:
                        key = "d_" + d.dsem
                        sem, val = me.dsems[d.dsem][0], d.dval
                    else:
                        key = "e_" + d.eng
                        sem, val = me.esem[d.eng], d.sigval
                    if waited.get(key, 0) < val:
                        eng.wait_ge(sem, val)
                        waited[key] = val
                if o.fn is None:
                    continue
                ins = o.fn(eng)
                if o.dsem is not None:
                    ins.then_inc(me.dsems[o.dsem][0], 16)
                elif o.sig:
                    ins.then_inc(me.esem[engname], 1)
            for (fe, name) in me.final:
                if fe == engname:
                    eng.wait_ge(me.dsems[name][0], me.dsems[name][1])

        block.tensor(lambda e: run("pe", e))
        block.scalar(lambda e: run("act", e))
        block.vector(lambda e: run("dve", e))
        block.gpsimd(lambda e: run("pool", e))
        block.sync(lambda e: run("sp", e))


class Arena:
    def __init__(self, nc, es, nwords, name="arena"):
        self.t = es.enter_context(nc.sbuf_tensor(name, [128, nwords], F32))
        self.n = nwords
        self.off = 0
        self.hi = 0

    def mark(self):
        return self.off

    def release(self, m):
        self.off = m

    def alloc(self, shape, dtype):
        free = 1
        for s in shape[1:]:
            free *= s
        words = (free + 1) // 2 if dtype == BF16 else free
        words = (words + 7) // 8 * 8
        assert self.off + words <= self.n, f"arena overflow {self.off}+{words}>{self.n}"
        ap = self.t[0:shape[0], self.off:self.off + words]
        self.off += words
        self.hi = max(self.hi, self.off)
        if dtype == BF16:
            ap = ap.bitcast(BF16)[:, 0:free]
        elif dtype == F32:
            ap = ap[:, 0:free]
        else:
            ap = ap.bitcast(dtype)[:, 0:free]
        if len(shape) == 3:
            ap = ap.rearrange("p (a b) -> p a b", a=shape[1])
        elif len(shape) == 4:
            ap = ap.rearrange("p (a b c) -> p a b c", a=shape[1], b=shape[2])
        return ap


def build_program(debug=None, stop_after=None):
    debug = debug or []
    nc = bass.Bass("TRN2", target_bir_lowering=False)

    def din(name, shape):
        return nc.dram_tensor(name, shape, F32, kind="ExternalInput").ap()

    x = din("x", [2048, 1024])
    c = din("c", [8, 128])
    w_ada = din("w_ada", [1024, 6144])
    b_ada = din("b_ada", [1, 6144])
    g_norm1 = din("g_norm1", [1, 1024])
    w_in = din("w_in", [1024, 6160])
    w_dw = din("w_dw", [31, 512])
    b_dw = din("b_dw", [4, 128])
    g_conv_ln = din("g_conv_ln", [4, 128])
    b_conv_ln = din("b_conv_ln", [4, 128])
    w_conv_pw = din("w_conv_pw", [512, 1024])
    b_conv_pw = din("b_conv_pw", [8, 128])
    w_a2 = din("w_a2", [16, 512])
    b_a2 = din("b_a2", [1, 512])
    g_gla_norm = din("g_gla_norm", [1, 1024])
    w_gla_o = din("w_gla_o", [1024, 1024])
    w_out = din("w_out", [1024, 1024])
    g_norm2 = din("g_norm2", [1, 1024])
    w_router_g = din("w_router_g", [1024, 4])
    b_router_g = din("b_router_g", [1, 4])
    w_router_e = din("w_router_e", [1024, 32])
    b_router_e = din("b_router_e", [1, 32])
    wpack = din("wpack", [NE * 128, 12288])
    w_ada_f = din("w_ada_f", [1024, 2048])
    b_ada_f = din("b_ada_f", [1, 2048])
    g_final = din("g_final", [1, 1024])
    out = nc.dram_tensor("out", [2048, 1024], F32, kind="ExternalOutput").ap()
    h_scr = nc.dram_tensor("h_scr", [2048, 1024], F32).ap()
    u2_scr = nc.dram_tensor("u2_scr", [2048, 1024], BF16).ap()
    X_scr = nc.dram_tensor("X_scr", [NSLOT, 1024], BF16).ap()
    Y_scr = nc.dram_tensor("Y_scr", [NSLOT, 1024], F32).ap()
    dbg_outs = {}

    with ExitStack() as es:
        P = Prog(nc, es)
        A = Arena(nc, es, 52800)
        ps = es.enter_context(nc.psum_tensor("ps", [128, 8, 512], F32))

        def bankf(b):
            return ps[:, b, :]

        def bankb(b):
            return ps[:, b, :].bitcast(BF16)

        def MM(out_ap, pairs, r, w):
            def fn(e):
                n = len(pairs)
                ins = None
                for i, (l, rh) in enumerate(pairs):
                    ins = e.matmul(out_ap, lhsT=l, rhs=rh, start=(i == 0), stop=(i == n - 1))
                return ins
            return P.add("pe", fn, r=r, w=w)

        def TR(outs_ins, ident_ap, r, w):
            def fn(e):
                ins = None
                for (o, i) in outs_ins:
                    ins = e.transpose(out=o, in_=i, identity=ident_ap)
                return ins
            return P.add("pe", fn, r=r, w=w)

        def ACT(out_ap, in_ap, func, r, w, **kw):
            return P.add("act", lambda e: e.activation(out=out_ap, in_=in_ap, func=func, **kw), r=r, w=w)

        def TT(eng, out_ap, in0, in1, op, r, w):
            return P.add(eng, lambda e: e.tensor_tensor(out=out_ap, in0=in0, in1=in1, op=op), r=r, w=w)

        def TS(eng, out_ap, in0, s1, s2, op0, op1, r, w):
            return P.add(eng, lambda e: e.tensor_scalar(out=out_ap, in0=in0, scalar1=s1, scalar2=s2, op0=op0, op1=op1), r=r, w=w)

        def TS1(eng, out_ap, in0, s1, op, r, w):
            return P.add(eng, lambda e: e.tensor_single_scalar(out=out_ap, in_=in0, scalar=s1, op=op), r=r, w=w)

        def STT(eng, out_ap, in0, scalar, in1, op0, op1, r, w):
            return P.add(eng, lambda e: e.scalar_tensor_tensor(out=out_ap, in0=in0, scalar=scalar, in1=in1, op0=op0, op1=op1), r=r, w=w)

        def CP(eng, out_ap, in_ap, r, w):
            return P.add(eng, lambda e: e.tensor_copy(out=out_ap, in_=in_ap), r=r, w=w)

        def MS(eng, ap, val, w):
            return P.add(eng, lambda e: e.memset(ap, val), w=w)

        def DMA(eng, out_ap, in_ap, r, w, dsem):
            return P.add(eng, lambda e: e.dma_start(out=out_ap, in_=in_ap), r=r, w=w, dsem=dsem)

        def RSTD(out_ap, ss_ap, n, r, w):
            ACT(out_ap, ss_ap, AF.Ln, r=r, w=w, scale=1.0 / n, bias=EPS)
            ACT(out_ap, out_ap, AF.Exp, r=w, w=w, scale=-0.5)

        def DBG(name, ap, shape, r):
            if name not in debug:
                return
            d = nc.dram_tensor("dbg_" + name, shape, F32, kind="ExternalOutput").ap()
            dbg_outs[name] = d
            DMA("pool", d, ap, r=r, w=["dbg_" + name], dsem="dbg")

        modB = A.alloc([128, 3, 1024], F32)
        _mod = {}

        def mod(m, lo=0, hi=1024):
            return (_mod["A"][:, m, lo:hi] if m < 5 else modB[:, m - 5, lo:hi])
        logits = A.alloc([128, NT, 36], F32)
        ss3 = A.alloc([128, NT], F32)
        rstd3 = A.alloc([128, NT], F32)
        identb = A.alloc([128, 128], BF16)
        ones_bf = A.alloc([128, 128], BF16)
        phase_mark = A.mark()
        _mod["A"] = A.alloc([128, 5, 1024], F32)
        identf = A.alloc([128, 128], F32)
        mask4 = A.alloc([128, 4, 128], F32)
        resetm = A.alloc([128, 512], F32)
        vecs = A.alloc([128, 28], F32)
        wdwT = A.alloc([128, 4, 32], F32)
        cT = A.alloc([128, 8], BF16)
        crep = A.alloc([128, 8, 128], BF16)
        wa1 = A.alloc([128, 8, 16], BF16)
        wa2aug = A.alloc([17, 512], BF16)
        a1T = A.alloc([17, 512], BF16)
        g_gla_bc = A.alloc([128, 1024], F32)
        wr_bf = A.alloc([128, 8, 36], BF16)
        br_bc = A.alloc([128, 36], F32)
        ss1 = A.alloc([128, NT], F32)
        rstd1 = A.alloc([128, NT], F32)
        ss2 = A.alloc([128, NT], F32)
        rstd2 = A.alloc([128, NT], F32)
        zT = A.alloc([128, 4, 542], BF16)
        S32 = A.alloc([128, 4, 256], F32)
        S16 = A.alloc([128, 4, 256], BF16)
        stage_w = A.alloc([31, 512], F32)
        stage_v = A.alloc([28, 128], F32)

        MS("pool", identf, 0.0, w=["identf"])
        P.add("pool", lambda e: e.affine_select(out=identf, in_=identf, pattern=[[-1, 128]], compare_op=ALU.not_equal,
                                                fill=1.0, base=0, channel_multiplier=1), r=["identf"], w=["identf"])
        CP("pool", identb, identf, r=["identf"], w=["identb"])
        MS("pool", ones_bf, 1.0, w=["ones_bf"])
        MS("pool", mask4, 1.0, w=["mask4"])
        P.add("pool", lambda e: e.affine_select(out=mask4, in_=mask4, pattern=[[0, 4], [1, 128]], compare_op=ALU.is_ge,
                                                fill=0.0, base=0, channel_multiplier=-1), r=["mask4"], w=["mask4"])
        MS("pool", resetm, 1.0, w=["resetm"])
        for t in range(4):
            MS("pool", resetm[:, t * 128:t * 128 + 1], 0.0, w=["resetm"])
        MS("pool", a1T, 1.0, w=["a1T"])
        MS("pool", zT, 0.0, w=["zT"])

        DMA("sp", stage_w, w_dw[:, :], r=[], w=["stage_w"], dsem="c_sw")
        DMA("sp", stage_v[0:4, :], b_dw[:, :], r=[], w=["stage_v"], dsem="c_sv")
        DMA("sp", stage_v[4:8, :], g_conv_ln[:, :], r=[], w=["stage_v"], dsem="c_sv")
        DMA("sp", stage_v[8:12, :], b_conv_ln[:, :], r=[], w=["stage_v"], dsem="c_sv")
        DMA("sp", stage_v[12:20, :], b_conv_pw[:, :], r=[], w=["stage_v"], dsem="c_sv")
        DMA("sp", stage_v[20:28, :], c[:, :], r=[], w=["stage_v"], dsem="c_sv")
        DMA("sp", g_gla_bc, g_gla_norm.partition_broadcast(128), r=[], w=["g_gla_bc"], dsem="c_gg")
        DMA("sp", br_bc[:, 0:4], b_router_g.partition_broadcast(128), r=[], w=["br_bc"], dsem="c_br")
        DMA("sp", br_bc[:, 4:36], b_router_e.partition_broadcast(128), r=[], w=["br_bc"], dsem="c_br")
        with nc.allow_non_contiguous_dma(reason="small router/a1 weights"):
            DMA("pool", wa1, w_in[:, 4096:4112].rearrange("(k p) f -> p k f", p=128), r=[], w=["wa1"], dsem="c_wa1")
            DMA("pool", wr_bf[:, :, 0:4], w_router_g.rearrange("(k p) f -> p k f", p=128), r=[], w=["wr_bf"], dsem="c_wr")
            DMA("pool", wr_bf[:, :, 4:36], w_router_e.rearrange("(k p) f -> p k f", p=128), r=[], w=["wr_bf"], dsem="c_wr")
        DMA("pool", wa2aug[0:16, :], w_a2[:, :], r=[], w=["wa2aug"], dsem="c_wa2")
        DMA("pool", wa2aug[16:17, :], b_a2[:, :], r=[], w=["wa2aug"], dsem="c_wa2")


        NWB = 3
        wbuf = [A.alloc([128, 8, 512], BF16) for _ in range(NWB)]
        junk = A.alloc([128, 1024], BF16)
        tmpA = A.alloc([128, 1024], F32)
        ut = [A.alloc([128, 1024], BF16) for _ in range(2)]
        uT_q = A.alloc([128, 8, 512], BF16)
        m0T = A.alloc([128, 8, 512], BF16)
        qT = A.alloc([128, 4, 512], BF16)
        kT = A.alloc([128, 4, 512], BF16)
        v_q = A.alloc([128, 4, 1024], BF16)
        sr_q = A.alloc([128, 4, 1024], BF16)
        g0s = A.alloc([128, 8, 512], BF16)
        ebl = A.alloc([128, 4, 4], F32)
        oss = A.alloc([128, 4], F32)
        orstd = A.alloc([128, 4], F32)
        og = [A.alloc([128, 1024], BF16) for _ in range(2)]
        ogT_q = A.alloc([128, 8, 512], BF16)
        xr = [A.alloc([128, 1024], F32) for _ in range(2)]
        xt = xr[0]
        attnT = A.alloc([128, 4, 128], BF16)
        ztile = A.alloc([128, 512], BF16)
        zc = A.alloc([128, 4, 512], BF16)
        zsq = A.alloc([128, 4, 512], BF16)
        diag = A.alloc([128, 31, 128], BF16)
        sgt = [A.alloc([128, 512], BF16) for _ in range(2)]
        mean_sb = A.alloc([128, 512], F32)
        rstd_sb = A.alloc([128, 512], F32)
        lnt = A.alloc([128, 512], F32)
        klT = A.alloc([128, 4, 512], BF16)
        kl = A.alloc([128, 4, 4, 128], BF16)
        gX = [A.alloc([128, 512], F32) for _ in range(2)]
        gY = [A.alloc([128, 512], F32) for _ in range(2)]
        print("arena words used (mixer phase):", A.mark())

        sw_hi, sw_lo = junk[0:31, 0:512], junk[0:31, 512:1024]
        sv_hi, sv_lo = og[0][0:28, 0:128], og[0][0:28, 128:256]
        CP("dve", sw_hi, stage_w, r=["stage_w"], w=["junk"])
        TT("dve", sw_lo, stage_w, sw_hi, ALU.subtract, r=["stage_w", "junk"], w=["junk"])
        CP("dve", sv_hi, stage_v, r=["stage_v"], w=["og0"])
        TT("dve", sv_lo, stage_v, sv_hi, ALU.subtract, r=["stage_v", "og0"], w=["og0"])
        bh, bl = P.nb(), P.nb()
        TR([(bankb(bh)[:, cc * 32:cc * 32 + 31], sw_hi[:, cc * 128:(cc + 1) * 128]) for cc in range(4)], identb[0:31, 0:31],
           r=["junk", "identb"], w=[f"ps{bh}"])
        TR([(bankb(bl)[:, cc * 32:cc * 32 + 31], sw_lo[:, cc * 128:(cc + 1) * 128]) for cc in range(4)], identb[0:31, 0:31],
           r=["junk", "identb"], w=[f"ps{bl}"])
        MS("dve", wdwT, 0.0, w=["wdwT"])
        CP("dve", wdwT[:, :, 0:31], bankb(bh)[:, 0:128].rearrange("p (a b) -> p a b", a=4)[:, :, 0:31], r=[f"ps{bh}"], w=["wdwT"])
        TT("dve", wdwT[:, :, 0:31], wdwT[:, :, 0:31], bankb(bl)[:, 0:128].rearrange("p (a b) -> p a b", a=4)[:, :, 0:31], ALU.add,
           r=[f"ps{bl}", "wdwT"], w=["wdwT"])
        bh, bl = P.nb(), P.nb()
        TR([(bankb(bh)[:, 0:28], sv_hi)], identb[0:28, 0:28], r=["og0", "identb"], w=[f"ps{bh}"])
        TR([(bankb(bl)[:, 0:28], sv_lo)], identb[0:28, 0:28], r=["og0", "identb"], w=[f"ps{bl}"])
        CP("dve", vecs, bankb(bh)[:, 0:28], r=[f"ps{bh}"], w=["vecs"])
        TT("dve", vecs, vecs, bankb(bl)[:, 0:28], ALU.add, r=[f"ps{bl}", "vecs"], w=["vecs"])
        ACT(cT, vecs[:, 20:28], AF.Silu, r=["vecs"], w=["cT"])
        CP("dve", crep, cT.unsqueeze(2).to_broadcast([128, 8, 128]), r=["cT"], w=["crep"])

        wcnt = [0]
        zf_state = [0, 0]

        def wload(src_ap, shape3=None):
            s = wcnt[0] % NWB
            wcnt[0] += 1
            dst = wbuf[s]
            if shape3 is not None:
                dst = dst.rearrange("p a b -> p (a b)").rearrange("p (a b) -> p a b", a=shape3[0])
            DMA("pool", dst, src_ap, r=[], w=[f"wb{s}"], dsem=f"wb{s}")
            return dst, f"wb{s}"

        bb = [(lnt, "lnt"), (mean_sb, "mean_sb")]

        def ADA(j, nbf):
            src = w_ada[:, j * 512:(j + 1) * 512] if j < 12 else w_ada_f[:, (j - 12) * 512:(j - 11) * 512]
            bsrc = b_ada[:, j * 512:(j + 1) * 512] if j < 12 else b_ada_f[:, (j - 12) * 512:(j - 11) * 512]
            wb_, rwb = wload(src.rearrange("(k p) f -> p k f", p=128))
            bbuf, bname = bb[j % 2]
            DMA("sp", bbuf, bsrc.partition_broadcast(128), r=[], w=[bname], dsem=f"bb{j % 2}")
            b = nbf()
            MM(bankf(b), [(crep[:, kc, :], wb_[:, kc, :]) for kc in range(8)], r=["crep", rwb], w=[f"ps{b}"])
            m, hf = j // 2, j % 2
            TT("dve", mod(m, hf * 512, (hf + 1) * 512), bankf(b), bbuf, ALU.add,
               r=[f"ps{b}", bname], w=[f"mod{m}"])

        def GSCALE(m, gsrc):
            DMA("sp", tmpA, gsrc.partition_broadcast(128), r=[], w=["tmpA"], dsem="gld")
            STT("dve", mod(m), mod(m), 1.0, tmpA, ALU.add, ALU.mult, r=[f"mod{m}", "tmpA"], w=[f"mod{m}"])

        for j in range(4):
            ADA(j, P.nb)
        GSCALE(1, g_norm1)

        wcnt = [0]
        zf_state = [0, 0]

        def wload(src_ap, shape3=None):
            s = wcnt[0] % NWB
            wcnt[0] += 1
            dst = wbuf[s]
            if shape3 is not None:
                dst = dst.rearrange("p a b -> p (a b)").rearrange("p (a b) -> p a b", a=shape3[0])
            DMA("pool", dst, src_ap, r=[], w=[f"wb{s}"], dsem=f"wb{s}")
            return dst, f"wb{s}"

        def win(c0):
            return wload(w_in[:, c0:c0 + 512].rearrange("(k p) f -> p k f", p=128))

        def bank_cycle(lst):
            st = [0]

            def nxt():
                b = lst[st[0] % len(lst)]
                st[0] += 1
                return b
            return nxt

        def zipper(chain, heavy):
            for i in range(max(len(chain), len(heavy))):
                if i < len(chain):
                    chain[i]()
                if i < len(heavy):
                    heavy[i]()

        for q in range(NQ):
            def PA_a(t, q=q):
                gt = 4 * q + t
                xx = xr[gt % 2]
                rx_ = f"xr{gt % 2}"
                DMA("sp", xx, x[gt * 128:(gt + 1) * 128, :], r=[], w=[rx_], dsem=rx_)
                ACT(junk, xx, AF.Square, r=[rx_], w=["junk", f"ss1_{gt}"], accum_out=ss1[:, gt:gt + 1])
                RSTD(rstd1[:, gt:gt + 1], ss1[:, gt:gt + 1], 1024.0, r=[f"ss1_{gt}"], w=[f"rstd1_{gt}"])

            def PA_b(t, q=q):
                gt = 4 * q + t
                xx = xr[gt % 2]
                rx_ = f"xr{gt % 2}"
                STT("dve", tmpA, xx, rstd1[:, gt:gt + 1], mod(1), ALU.mult, ALU.mult,
                    r=[rx_, f"rstd1_{gt}", "mod1"], w=["tmpA"])
                u = ut[gt % 2]
                ru = f"ut{gt % 2}"
                TT("dve", u, tmpA, mod(0), ALU.add, r=["tmpA", "mod0"], w=[ru])
                b = P.nb()
                pT = bankb(b).rearrange("p (a b) -> p a b", a=8)
                TR([(pT[:, kc, :], u[:, kc * 128:(kc + 1) * 128]) for kc in range(8)], identb,
                   r=[ru, "identb"], w=[f"ps{b}"])
                ACT(uT_q[:, :, t * 128:(t + 1) * 128], pT, AF.Copy, r=[f"ps{b}"], w=["uT_q"])

            def B1():
                b = P.nb()
                MM(bankf(b)[0:16, :], [(wa1[:, kc, :], uT_q[:, kc, :]) for kc in range(8)], r=["wa1", "uT_q"], w=[f"ps{b}"])
                ACT(a1T[0:16, :], bankf(b)[0:16, :], AF.Copy, r=[f"ps{b}"], w=["a1T"])

            def Qp(nbf):
                wq_, rq = win(1024)
                for mc in range(4):
                    b = nbf()
                    MM(bankf(b), [(wq_[:, kc, mc * 128:(mc + 1) * 128], uT_q[:, kc, :]) for kc in range(8)], r=[rq, "uT_q"], w=[f"ps{b}"])
                    ACT(qT[:, mc, :], bankf(b), AF.Copy, r=[f"ps{b}"], w=[f"qT{mc}"], scale=float(128 ** -0.5))

            def Kp(nbf):
                wk_, rk = win(1536)
                for mc in range(4):
                    b = nbf()
                    MM(bankf(b), [(wk_[:, kc, mc * 128:(mc + 1) * 128], uT_q[:, kc, :]) for kc in range(8)], r=[rk, "uT_q"], w=[f"ps{b}"])
                    CP("dve", kT[:, mc, :], bankf(b), r=[f"ps{b}"], w=[f"kT{mc}"])

            def GATE_a(h, nbf):
                gx, gy = gX[h % 2], gY[h % 2]
                rx, ry = f"gX{h % 2}", f"gY{h % 2}"
                b = nbf()
                MM(bankf(b), [(wa2aug[:, h * 128:(h + 1) * 128], a1T)], r=["wa2aug", "a1T"], w=[f"ps{b}"])
                ACT(gx, bankf(b), AF.Exp, r=[f"ps{b}"], w=[rx], scale=-1.0)
                ACT(gx, gx, AF.Ln, r=[rx], w=[rx], bias=1.0)
                P.add("dve", (lambda gx=gx, gy=gy: (lambda e: e.tensor_tensor_scan(out=gy, data0=resetm, data1=gx, initial=0.0,
                                                                                    op0=ALU.mult, op1=ALU.add)))(), r=[rx, "resetm"], w=[ry])
                ACT(gx, gy, AF.Exp, r=[ry], w=[rx], scale=-1.0 / 16)
                ACT(gy, gy, AF.Exp, r=[ry], w=[ry], scale=1.0 / 16)

            def GATE_b(h):
                gx, gy = gX[h % 2], gY[h % 2]
                rx, ry = f"gX{h % 2}", f"gY{h % 2}"
                TT("dve", qT[:, h, :], qT[:, h, :], gx, ALU.mult, r=[f"qT{h}", rx], w=[f"qT{h}"])
                TT("dve", kT[:, h, :], kT[:, h, :], gy, ALU.mult, r=[f"kT{h}", ry], w=[f"kT{h}"])
                CP("dve", ebl[:, h, :], gx.rearrange("p (t c) -> p t c", t=4)[:, :, 127], r=[rx], w=[f"ebl{h}"])
                for t in range(4):
                    TS1("dve", klT[:, h, t * 128:(t + 1) * 128], kT[:, h, t * 128:(t + 1) * 128], ebl[:, h, t:t + 1],
                        ALU.mult, r=[f"kT{h}", f"ebl{h}"], w=[f"klT{h}"])

            def KL(nbf):
                for t in range(4):
                    b = nbf()
                    pT = bankb(b).rearrange("p (a b) -> p a b", a=8)
                    TR([(pT[:, h, :], klT[:, h, t * 128:(t + 1) * 128]) for h in range(4)], identb,
                       r=[f"klT{h}" for h in range(4)] + ["identb"], w=[f"ps{b}"])
                    ACT(kl[:, t, :, :], pT[:, 0:4, :], AF.Copy, r=[f"ps{b}"], w=[f"kl{t}"])

            def VP(hf, nbf):
                wv_, rv = win(2048 + hf * 512)
                for t in range(4):
                    b = nbf()
                    MM(bankf(b), [(uT_q[:, kc, t * 128:(t + 1) * 128], wv_[:, kc, :]) for kc in range(8)], r=[rv, "uT_q"], w=[f"ps{b}"])
                    CP("dve", v_q[:, t, hf * 512:(hf + 1) * 512], bankf(b), r=[f"ps{b}"], w=["v_q"])

            def RP(hf, nbf):
                wr_, rr = win(3072 + hf * 512)
                for t in range(4):
                    b = nbf()
                    MM(bankf(b), [(uT_q[:, kc, t * 128:(t + 1) * 128], wr_[:, kc, :]) for kc in range(8)], r=[rr, "uT_q"], w=[f"ps{b}"])
                    ACT(sr_q[:, t, hf * 512:(hf + 1) * 512], bankf(b), AF.Silu, r=[f"ps{b}"], w=["sr_q"])

            def CI(nbf):
                wa_, ra = win(0)
                wg_, rg = win(512)
                for mc in range(4):
                    ba, bg = nbf(), nbf()
                    MM(bankf(ba), [(wa_[:, kc, mc * 128:(mc + 1) * 128], uT_q[:, kc, :]) for kc in range(8)], r=[ra, "uT_q"], w=[f"ps{ba}"])
                    MM(bankf(bg), [(wg_[:, kc, mc * 128:(mc + 1) * 128], uT_q[:, kc, :]) for kc in range(8)], r=[rg, "uT_q"], w=[f"ps{bg}"])
                    ACT(sgt[mc % 2], bankf(bg), AF.Sigmoid, r=[f"ps{bg}"], w=[f"sgt{mc % 2}"])
                    TT("dve", zT[:, mc, 30:542], bankf(ba), sgt[mc % 2], ALU.mult, r=[f"ps{ba}", f"sgt{mc % 2}"], w=[f"zT{mc}"])

            def CVb(cc):
                for k in range(31):
                    TS1("dve", diag[:, k, :], identb, wdwT[:, cc, k:k + 1], ALU.mult, r=["identb", "wdwT"], w=[f"dg{k}"])

            def CVm(cc, nbf):
                b = nbf()
                for (k0, k1) in ((0, 16), (16, 31)):
                    P.add("pe", (lambda k0=k0, k1=k1, b=b, cc=cc: (lambda e: [e.matmul(bankf(b), lhsT=diag[:, k, :], rhs=zT[:, cc, k:k + 512],
                                                                                       start=(k == 0), stop=(k == 30))
                                                                              for k in range(k0, k1)][-1]))(),
                          r=[f"dg{k}" for k in range(k0, k1)] + [f"zT{cc}"], w=[f"ps{b}"])
                ACT(zc[:, cc, :], bankf(b), AF.Identity, r=[f"ps{b}", "vecs"], w=[f"zc{cc}"], bias=vecs[:, cc:cc + 1])
                ACT(zsq[:, cc, :], bankf(b), AF.Square, r=[f"ps{b}", "vecs"], w=[f"zsq{cc}"], bias=vecs[:, cc:cc + 1])
                CP("dve", zT[:, cc, 0:30], zT[:, cc, 512:542], r=[f"zT{cc}"], w=[f"zT{cc}"])

            def LN(nbf):
                bm, bq = nbf(), nbf()
                MM(bankf(bm), [(ones_bf, zc[:, cc, :]) for cc in range(4)], r=["ones_bf"] + [f"zc{cc}" for cc in range(4)], w=[f"ps{bm}"])
                MM(bankf(bq), [(ones_bf, zsq[:, cc, :]) for cc in range(4)], r=["ones_bf"] + [f"zsq{cc}" for cc in range(4)], w=[f"ps{bq}"])
                TS1("dve", mean_sb, bankf(bm), 1.0 / 512, ALU.mult, r=[f"ps{bm}"], w=["mean_sb"])
                TT("dve", lnt, mean_sb, mean_sb, ALU.mult, r=["mean_sb"], w=["lnt"])
                STT("dve", rstd_sb, bankf(bq), 1.0 / 512, lnt, ALU.mult, ALU.subtract, r=[f"ps{bq}", "lnt"], w=["rstd_sb"])
                ACT(rstd_sb, rstd_sb, AF.Ln, r=["rstd_sb"], w=["rstd_sb"], bias=EPS)
                ACT(rstd_sb, rstd_sb, AF.Exp, r=["rstd_sb"], w=["rstd_sb"], scale=-0.5)
                for cc in range(4):
                    TT("dve", lnt, zc[:, cc, :], mean_sb, ALU.subtract, r=[f"zc{cc}", "mean_sb"], w=["lnt"])
                    TT("dve", lnt, lnt, rstd_sb, ALU.mult, r=["lnt", "rstd_sb"], w=["lnt"])
                    ACT(zsq[:, cc, :], lnt, AF.Silu, r=["lnt", "vecs"], w=[f"zsq{cc}"], scale=vecs[:, 4 + cc:5 + cc], bias=vecs[:, 8 + cc:9 + cc])

            gsig = [gX[0], gX[1]]

            gs_w = {}

            def GS(col0, hf, nbf, mcs=(0, 1, 2, 3)):
                if (col0, hf) not in gs_w:
                    gs_w[(col0, hf)] = win(col0 + hf * 512)
                wg0, rg0 = gs_w[(col0, hf)]
                for mc in mcs:
                    b = nbf()
                    MM(bankf(b), [(wg0[:, kc, mc * 128:(mc + 1) * 128], uT_q[:, kc, :]) for kc in range(8)], r=[rg0, "uT_q"], w=[f"ps{b}"])
                    ACT(gsig[mc % 2], bankf(b), AF.Exp, r=[f"ps{b}"], w=[f"gX{mc % 2}"], scale=-1.0)
                    ACT(gsig[mc % 2], gsig[mc % 2], AF.Ln, r=[f"gX{mc % 2}"], w=[f"gX{mc % 2}"], bias=1.0)
                    ACT(g0s[:, hf * 4 + mc, :], gsig[mc % 2], AF.Exp, r=[f"gX{mc % 2}"], w=["g0s"], scale=-1.0)

            pw_w = {}

            def PW(nbf, dchs=range(8)):
                if "w" not in pw_w:
                    pw_w["w"] = wload(w_conv_pw.rearrange("(k p) f -> p k f", p=128), shape3=(4, 1024))
                wpw, rpw = pw_w["w"]
                for dch in dchs:
                    b = nbf()
                    MM(bankf(b), [(wpw[:, cc, dch * 128:(dch + 1) * 128], zsq[:, cc, :]) for cc in range(4)], r=[rpw] + [f"zsq{cc}" for cc in range(4)], w=[f"ps{b}"])
                    STT("dve", m0T[:, dch, :], bankf(b), vecs[:, 12 + dch:13 + dch], g0s[:, dch, :], ALU.add, ALU.mult,
                        r=[f"ps{b}", "vecs", "g0s"], w=["m0T"])

            BA, BO, BS = 0, (1, 2), (3, 4)

            def oap_(h):
                return bankf(BO[h // 2])[:, (h % 2) * 256:(h % 2 + 1) * 256]

            def REC_front(t, q=q):
                gt = 4 * q + t
                tc = slice(t * 128, (t + 1) * 128)
                for h in range(4):
                    MM(bankf(BA)[:, h * 128:(h + 1) * 128], [(kT[:, h, tc], qT[:, h, tc])], r=[f"kT{h}", f"qT{h}"], w=[f"ps{BA}"])
                TT("dve", attnT, bankf(BA).rearrange("p (a b) -> p a b", a=4), mask4, ALU.mult, r=[f"ps{BA}", "mask4"], w=["attnT"])
                for h in range(4):
                    pairs = [(attnT[:, h, :], v_q[:, t, h * 256:(h + 1) * 256])]
                    if gt > 0:
                        pairs.append((qT[:, h, tc], S16[:, h, :]))
                    MM(oap_(h), pairs, r=["attnT", "v_q", f"qT{h}", "S16"], w=[f"ps{BO[h // 2]}"])
                if gt < NT - 1:
                    for h in range(4):
                        sap = bankf(BS[h // 2])[:, (h % 2) * 256:(h % 2 + 1) * 256]
                        MM(sap, [(kl[:, t, h, :], v_q[:, t, h * 256:(h + 1) * 256])], r=[f"kl{t}", "v_q"], w=[f"ps{BS[h // 2]}"])
                    for h in range(4):
                        sap = bankf(BS[h // 2])[:, (h % 2) * 256:(h % 2 + 1) * 256]
                        if gt == 0:
                            CP("dve", S32[:, h, :], sap, r=[f"ps{BS[h // 2]}"], w=["S32"])
                        else:
                            STT("dve", S32[:, h, :], S32[:, h, :], ebl[:, h, t:t + 1], sap, ALU.mult, ALU.add,
                                r=["S32", f"ebl{h}", f"ps{BS[h // 2]}"], w=["S32"])
                    ACT(S16, S32, AF.Copy, r=["S32"], w=["S16"])

            def REC_post1(t, q=q):
                gt = 4 * q + t
                for h in range(4):
                    ACT(junk[:, 0:256], oap_(h), AF.Square, r=[f"ps{BO[h // 2]}"], w=["junk", "oss"], accum_out=oss[:, h:h + 1])
                RSTD(orstd, oss, 256.0, r=["oss"], w=["orstd"])
                TT("dve", tmpA, g_gla_bc, sr_q[:, t, :], ALU.mult, r=["g_gla_bc", "sr_q"], w=["tmpA"])
                ogt = og[gt % 2]
                for h in range(4):
                    STT("dve", ogt[:, h * 256:(h + 1) * 256], oap_(h), orstd[:, h:h + 1], tmpA[:, h * 256:(h + 1) * 256], ALU.mult, ALU.mult,
                        r=[f"ps{BO[h // 2]}", "orstd", "tmpA"], w=[f"og{gt % 2}"])

            def REC_post2(t, q=q):
                gt = 4 * q + t
                tc = slice(t * 128, (t + 1) * 128)
                ogt = og[gt % 2]
                pT = bankb(BA).rearrange("p (a b) -> p a b", a=8)
                TR([(pT[:, ec, :], ogt[:, ec * 128:(ec + 1) * 128]) for ec in range(8)], identb, r=[f"og{gt % 2}", "identb"], w=[f"ps{BA}"])
                ACT(ogT_q[:, :, tc], pT, AF.Copy, r=[f"ps{BA}"], w=["ogT_q"])

            def GO(nbf):
                for hf in range(2):
                    wgo, rgo = wload(w_gla_o[:, hf * 512:(hf + 1) * 512].rearrange("(k p) f -> p k f", p=128))
                    for mc in range(4):
                        dch = hf * 4 + mc
                        b = nbf()
                        MM(bankf(b), [(wgo[:, ec, mc * 128:(mc + 1) * 128], ogT_q[:, ec, :]) for ec in range(8)], r=[rgo, "ogT_q"], w=[f"ps{b}"])
                        TT("dve", tmpA[:, 0:512], bankf(b), g0s[:, dch, :], ALU.mult, r=[f"ps{b}", "g0s"], w=["tmpA"])
                        TT("dve", m0T[:, dch, :], tmpA[:, 0:512], m0T[:, dch, :], ALU.add, r=["tmpA", "m0T"], w=["m0T"])

            wo = [None, None]

            ob = {}

            def OUT_mm(t, q=q):
                ob[t] = (P.nb(), P.nb())
                for hf in range(2):
                    b = ob[t][hf]
                    MM(bankf(b), [(m0T[:, kc, t * 128:(t + 1) * 128], wo[hf][0][:, kc, :]) for kc in range(8)], r=["m0T", wo[hf][1]], w=[f"ps{b}"])

            def OUT_res(t, q=q):
                gt = 4 * q + t
                xx = xr[gt % 2]
                rx_ = f"xr{gt % 2}"
                DMA("sp", xx, x[gt * 128:(gt + 1) * 128, :], r=[], w=[rx_], dsem=rx_)
                for hf in range(2):
                    b = ob[t][hf]
                    TT("dve", tmpA[:, hf * 512:(hf + 1) * 512], bankf(b), mod(2, hf * 512, (hf + 1) * 512), ALU.mult, r=[f"ps{b}", "mod2"], w=["tmpA"])
                    TT("dve", xx[:, hf * 512:(hf + 1) * 512], tmpA[:, hf * 512:(hf + 1) * 512], xx[:, hf * 512:(hf + 1) * 512], ALU.add,
                       r=["tmpA", rx_], w=[rx_])
                DMA("sp", h_scr[gt * 128:(gt + 1) * 128, :], xx, r=[rx_], w=[f"hscr{gt}"], dsem=f"hout{gt % 2}")
                ACT(junk, xx, AF.Square, r=[rx_], w=["junk", f"ss2_{gt}"], accum_out=ss2[:, gt:gt + 1])
                RSTD(rstd2[:, gt:gt + 1], ss2[:, gt:gt + 1], 1024.0, r=[f"ss2_{gt}"], w=[f"rstd2_{gt}"])

            lgb = {}

            def OUT_back(t, q=q):
                gt = 4 * q + t
                xx = xr[gt % 2]
                rx_ = f"xr{gt % 2}"
                STT("dve", tmpA, xx, rstd2[:, gt:gt + 1], mod(4), ALU.mult, ALU.mult,
                    r=[rx_, f"rstd2_{gt}", "mod4"], w=["tmpA"])
                u2hi = ut[gt % 2]
                ru = f"ut{gt % 2}"
                TT("dve", u2hi, tmpA, mod(3), ALU.add, r=["tmpA", "mod3"], w=[ru])
                b = P.nb()
                pT = bankb(b).rearrange("p (a b) -> p a b", a=8)
                TR([(pT[:, kc, :], u2hi[:, kc * 128:(kc + 1) * 128]) for kc in range(8)], identb, r=[ru, "identb"], w=[f"ps{b}"])
                u2t = og[gt % 2].rearrange("p (a b) -> p a b", a=8)
                ACT(u2t, pT, AF.Copy, r=[f"ps{b}"], w=[f"og{gt % 2}"])
                DMA("sp", u2_scr[gt * 128:(gt + 1) * 128, :], u2hi, r=[ru], w=[f"u2scr{gt}"], dsem=f"u2out{gt % 2}")
                b = P.nb()
                lgb[t] = b
                MM(bankf(b)[:, 0:36], [(u2t[:, kc, :], wr_bf[:, kc, :]) for kc in range(8)], r=[f"og{gt % 2}", "wr_bf"], w=[f"ps{b}"])

            def OUT_la(t, q=q):
                gt = 4 * q + t
                b = lgb[t]
                TT("dve", logits[:, gt, :], bankf(b)[:, 0:36], br_bc, ALU.add, r=[f"ps{b}", "br_bc"], w=["logits"])

            PA_a(0)
            PA_a(1)
            for t in range(4):
                PA_b(t)
                if t + 2 < 4:
                    PA_a(t + 2)
            B1()
            nb1 = bank_cycle([2, 3, 4, 5, 6, 7])
            nbc = bank_cycle([0, 1])
            zipper([lambda: GATE_a(0, nbc), lambda: GATE_a(1, nbc), lambda: (GATE_b(0), GATE_a(2, nbc)), lambda: (GATE_b(1), GATE_a(3, nbc)),
                    lambda: GATE_b(2), lambda: (GATE_b(3), KL(nbc))],
                   [lambda: Qp(nb1), lambda: Kp(nb1), lambda: VP(0, nb1), lambda: VP(1, nb1)]
                   + ([lambda: ADA(4 + 6 * q, nb1), lambda: ADA(5 + 6 * q, nb1)] if q < 2 else []))
            CVb(0)
            CI(P.nb)
            for cc in range(4):
                CVm(cc, P.nb)
                if cc + 1 < 4:
                    CVb(cc + 1)
            LN(P.nb)
            RP(0, P.nb)
            RP(1, P.nb)
            nb2 = bank_cycle([5, 6, 7])
            zf = [0]

            def ZF(n, q=q):
                if q == 0 and zf[0] == 0:
                    MS("dve", ztile, 0.0, w=["ztile"])
                for _ in range(n):
                    r0 = zf_state[0]
                    if r0 >= NSLOT:
                        return
                    na = min(8, (NSLOT - r0) // 128)
                    hf = zf_state[1]
                    DMA("sp", X_scr[r0:r0 + na * 128, hf * 512:(hf + 1) * 512].rearrange("(a p) d -> p a d", p=128),
                        ztile.unsqueeze(1).to_broadcast([128, na, 512]), r=["ztile"], w=[f"Xz{r0}_{hf}"], dsem="xz")
                    zf[0] += 1
                    if hf == 1:
                        zf_state[0] = r0 + na * 128
                    zf_state[1] = 1 - hf

            def ADAq(i, q=q):
                if q < 2:
                    ADA(6 + 6 * q + i, nb2)

            HA = [lambda: GS(4112, 0, nb2, (0, 1)), lambda: GS(4112, 1, nb2, (0, 1)), lambda: PW(nb2, range(0, 4)),
                  lambda: GS(5136, 0, nb2, (0, 1)), lambda: GS(5136, 1, nb2, (0, 1))]
            HB = [lambda: GS(4112, 0, nb2, (2, 3)), lambda: GS(4112, 1, nb2, (2, 3)), lambda: PW(nb2, range(4, 8)),
                  lambda: GS(5136, 0, nb2, (2, 3)), lambda: GS(5136, 1, nb2, (2, 3))]
            for i in range(5):
                if i < 4:
                    REC_front(i)
                if i >= 1:
                    REC_post2(i - 1)
                HA[i]()
                if i < 4:
                    REC_post1(i)
                HB[i]()
                ZF(2 if (i < 4 or q < 2) else 100)
                if i < 4:
                    ADAq(i)
            GO(P.nb)
            if q == 0:
                GSCALE(4, g_norm2)
            elif q == 1:
                GSCALE(7, g_final)
            wo[0] = wload(w_out[:, 0:512].rearrange("(k p) f -> p k f", p=128))
            wo[1] = wload(w_out[:, 512:1024].rearrange("(k p) f -> p k f", p=128))
            OUT_mm(0)
            OUT_res(0)
            OUT_mm(1)
            OUT_res(1)
            for t in range(4):
                if t + 2 < 4:
                    OUT_mm(t + 2)
                OUT_back(t)
                if t + 2 < 4:
                    OUT_res(t + 2)
                OUT_la(t)

        DBG("logits", logits.rearrange("p a b -> p (a b)"), [128, NT * 36], r=["logits"])

        P.barrier()
        A.release(phase_mark)
        slots_i = es.enter_context(nc.sbuf_tensor("slots_i", [128, NT * 2], I32))[:, :].rearrange("p (a b) -> p a b", a=NT)
        wt1 = A.alloc([128, NT], F32)
        wt2 = A.alloc([128, NT], F32)
        widx_i = es.enter_context(nc.sbuf_tensor("widx_i", [128, NOT], I32))[:, :]
        moe_tmp_mark = A.mark()
        rb_slot = es.enter_context(nc.gpsimd.register("rb_slot"))
        rb_w = es.enter_context(nc.gpsimd.register("rb_w"))
        P.add("pool", lambda e: (e.reg_mov(rb_slot, NSLOT - 1), e.reg_mov(rb_w, NE * 128 - 1))[-1])
        NWS = 3
        wS = [A.alloc([128, 12288], BF16) for _ in range(NWS + 2)]

        def wslot(ws):
            return ws % NWS if ws < NE else NWS + (ws - NE) % 2
        wE = [(w_[:, 0:4096].rearrange("p (a b) -> p a b", a=8), w_[:, 4096:8192].rearrange("p (a b) -> p a b", a=8),
               w_[:, 8192:12288].rearrange("p (a b) -> p a b", a=4)) for w_ in wS]
        route_mark = A.mark()
        u2tm = A.alloc([128, NT, 1024], BF16)
        Lst = A.alloc([128, 128], BF16)
        gmax = A.alloc([128, NT], F32)
        gmask = A.alloc([128, NT, 4], F32)
        eg = A.alloc([128, NT, 4], F32)
        gsum = A.alloc([128, NT], F32)
        wgt = A.alloc([128, NT], F32)
        lem = A.alloc([128, NT, NE], F32)
        pen = A.alloc([128, NT, NE], F32)
        mk1 = A.alloc([128, NT, NE], F32)
        mk2 = A.alloc([128, NT, NE], F32)
        m1 = A.alloc([128, NT], F32)
        m2 = A.alloc([128, NT], F32)
        dd = A.alloc([128, NT], F32)
        Mb = A.alloc([128, NT * NE], BF16)
        cs_ei = A.alloc([128, NE, NT], F32)
        resetE = A.alloc([128, NE, NT], F32)
        incl = A.alloc([128, NE, NT], F32)
        excl = A.alloc([128, NE, NT], F32)
        rank = A.alloc([128, NT, NE], F32)
        iota_i = es.enter_context(nc.sbuf_tensor("iota_i", [128, 32], I32))[:, :]
        iota_f = A.alloc([128, 32], F32)
        ones32 = A.alloc([128, 32], F32)
        o_e = A.alloc([128, NE], F32)
        thr14 = A.alloc([128, 14], F32)
        cmp14 = A.alloc([128, NE, 14], F32)
        ot_e = A.alloc([128, NE], F32)
        osz = A.alloc([128, NE], F32)
        oincl = A.alloc([128, NE], F32)
        obase = A.alloc([128, NE], F32)
        thrJ = A.alloc([128, NOT], F32)
        cmpJ = A.alloc([128, NOT, NE], F32)
        oe_f = A.alloc([128, NOT], F32)
        iotap_i = es.enter_context(nc.sbuf_tensor("iotap_i", [128, 1], I32))[:, :]
        iotap_f = A.alloc([128, 1], F32)
        eC = A.alloc([128, NE], F32)
        Bv = A.alloc([128, NE], F32)
        isov = A.alloc([128, NT, NE], F32)
        sval = A.alloc([128, NT, NE], F32)
        slots_f = A.alloc([128, NT, 2], F32)
        print("arena words used (moe phase):", A.mark())

        loaded = set()

        def load_weights(ws):
            if ws in loaded:
                return
            loaded.add(ws)
            s_ = wslot(ws)
            if ws < NE:
                DMA("pool", wS[s_], wpack[ws * 128:(ws + 1) * 128, :], r=[], w=[f"wE{s_}"], dsem=f"wE{s_}")
            else:
                j = ws - NE
                P.add("pool", (lambda j=j, s_=s_: (lambda e: e.indirect_dma_start(
                    out=wS[s_], out_offset=None, in_=wpack[:, :],
                    in_offset=bass.IndirectOffsetOnAxis(ap=widx_i[:, j:j + 1], axis=0),
                    bounds_check=rb_w, oob_is_err=False)))(), r=["widx_i"], w=[f"wE{s_}"], dsem=f"wE{s_}")

        load_weights(0)
        DMA("sp", u2tm, u2_scr.rearrange("(i p) d -> p i d", p=128), r=[], w=["u2tm"], dsem="u2in")
        MS("pool", Lst, 1.0, w=["Lst"])
        P.add("pool", lambda e: e.affine_select(out=Lst, in_=Lst, pattern=[[1, 128]], compare_op=ALU.is_ge,
                                                fill=0.0, base=-1, channel_multiplier=-1), r=["Lst"], w=["Lst"])
        P.add("pool", lambda e: e.iota(out=iota_i, pattern=[[1, 32]], base=0, channel_multiplier=0), w=["iota_i"])
        CP("dve", iota_f, iota_i, r=["iota_i"], w=["iota_f"])
        MS("dve", ones32, 1.0, w=["ones32"])
        MS("dve", resetE, 1.0, w=["resetE"])
        MS("dve", resetE[:, :, 0:1], 0.0, w=["resetE"])

        lg = logits[:, :, 0:4]
        le = logits[:, :, 4:36]
        P.add("dve", lambda e: e.tensor_reduce(out=gmax, in_=lg, axis=AX.X, op=ALU.max), r=["logits"], w=["gmax"])
        TT("dve", gmask, lg, gmax.unsqueeze(2).to_broadcast([128, NT, 4]), ALU.is_equal, r=["logits", "gmax"], w=["gmask"])
        TT("dve", eg, lg, gmax.unsqueeze(2).to_broadcast([128, NT, 4]), ALU.subtract, r=["logits", "gmax"], w=["eg"])
        ACT(eg, eg, AF.Exp, r=["eg"], w=["eg"])
        P.add("dve", lambda e: e.tensor_reduce(out=gsum, in_=eg, axis=AX.X, op=ALU.add), r=["eg"], w=["gsum"])
        P.add("dve", lambda e: e.reciprocal(out=wgt, in_=gsum), r=["gsum"], w=["wgt"])
        CP("dve", pen.rearrange("p t (g e) -> p t g e", g=4), gmask.unsqueeze(3).to_broadcast([128, NT, 4, 8]), r=["gmask"], w=["pen"])
        TS("dve", pen, pen, 1e30, -1e30, ALU.mult, ALU.add, r=["pen"], w=["pen"])
        TT("dve", lem, le, pen, ALU.add, r=["logits", "pen"], w=["lem"])
        P.add("dve", lambda e: e.tensor_reduce(out=m1, in_=lem, axis=AX.X, op=ALU.max), r=["lem"], w=["m1"])
        TT("dve", mk1, lem, m1.unsqueeze(2).to_broadcast([128, NT, NE]), ALU.is_equal, r=["lem", "m1"], w=["mk1"])
        STT("dve", lem, mk1, -1e30, lem, ALU.mult, ALU.add, r=["mk1", "lem"], w=["lem"])
        P.add("dve", lambda e: e.tensor_reduce(out=m2, in_=lem, axis=AX.X, op=ALU.max), r=["lem"], w=["m2"])
        TT("dve", mk2, lem, m2.unsqueeze(2).to_broadcast([128, NT, NE]), ALU.is_equal, r=["lem", "m2"], w=["mk2"])
        TT("dve", dd, m2, m1, ALU.subtract, r=["m1", "m2"], w=["dd"])
        ACT(dd, dd, AF.Exp, r=["dd"], w=["dd"])
        TS1("dve", dd, dd, 1.0, ALU.add, r=["dd"], w=["dd"])
        P.add("dve", lambda e: e.reciprocal(out=dd, in_=dd), r=["dd"], w=["dd"])
        TT("dve", wt1, wgt, dd, ALU.mult, r=["wgt", "dd"], w=["wt1"])
        TT("dve", wt2, wgt, wt1, ALU.subtract, r=["wgt", "wt1"], w=["wt2"])

        TT("dve", Mb.rearrange("p (t e) -> p t e", t=NT), mk1, mk2, ALU.add, r=["mk1", "mk2"], w=["Mb"])
        bR1, bR2 = P.nb(), P.nb()
        MM(bankf(bR1), [(Lst, Mb)], r=["Lst", "Mb"], w=[f"ps{bR1}"])
        MM(bankf(bR2), [(ones_bf, Mb)], r=["ones_bf", "Mb"], w=[f"ps{bR2}"])
        CP("dve", cs_ei, bankf(bR2).rearrange("p (i e) -> p e i", i=NT), r=[f"ps{bR2}"], w=["cs_ei"])
        P.add("dve", lambda e: e.tensor_tensor_scan(out=incl.rearrange("p e i -> p (e i)"), data0=resetE.rearrange("p e i -> p (e i)"),
                                                    data1=cs_ei.rearrange("p e i -> p (e i)"), initial=0.0, op0=ALU.mult, op1=ALU.add),
              r=["resetE", "cs_ei"], w=["incl"])
        TT("dve", excl, incl, cs_ei, ALU.subtract, r=["incl", "cs_ei"], w=["excl"])
        TT("dve", rank, bankf(bR1).rearrange("p (i e) -> p i e", i=NT), excl.rearrange("p e i -> p i e"), ALU.add,
           r=[f"ps{bR1}", "excl"], w=["rank"])
        n_e = incl[:, :, NT - 1]
        TS("dve", o_e, n_e, -float(CAP), 0.0, ALU.add, ALU.max, r=["incl"], w=["o_e"])
        TS1("dve", thr14, iota_f[:, 0:14], 128.0, ALU.mult, r=["iota_f"], w=["thr14"])
        TT("dve", cmp14, o_e.unsqueeze(2).to_broadcast([128, NE, 14]), thr14.unsqueeze(1).to_broadcast([128, NE, 14]), ALU.is_gt,
           r=["o_e", "thr14"], w=["cmp14"])
        P.add("dve", lambda e: e.tensor_reduce(out=ot_e, in_=cmp14, axis=AX.X, op=ALU.add), r=["cmp14"], w=["ot_e"])
        TS1("dve", osz, ot_e, 128.0, ALU.mult, r=["ot_e"], w=["osz"])
        P.add("dve", lambda e: e.tensor_tensor_scan(out=oincl, data0=ones32, data1=osz, initial=0.0, op0=ALU.mult, op1=ALU.add),
              r=["ones32", "osz"], w=["oincl"])
        TT("dve", obase, oincl, osz, ALU.subtract, r=["oincl", "osz"], w=["obase"])
        TS1("dve", thrJ, iota_f[:, 0:NOT], 128.0, ALU.mult, r=["iota_f"], w=["thrJ"])
        TT("dve", cmpJ, oincl.unsqueeze(1).to_broadcast([128, NOT, NE]), thrJ.unsqueeze(2).to_broadcast([128, NOT, NE]), ALU.is_le,
           r=["oincl", "thrJ"], w=["cmpJ"])
        P.add("dve", lambda e: e.tensor_reduce(out=oe_f, in_=cmpJ, axis=AX.X, op=ALU.add), r=["cmpJ"], w=["oe_f"])
        P.add("pool", lambda e: e.iota(out=iotap_i, pattern=[[0, 1]], base=0, channel_multiplier=1), w=["iotap_i"])
        CP("dve", iotap_f, iotap_i, r=["iotap_i"], w=["iotap_f"])
        TS("dve", cmpJ[:, 0, 0:NOT], oe_f, 128.0, iotap_f[:, 0:1], ALU.mult, ALU.add, r=["oe_f", "cmpJ", "iotap_f"], w=["cmpJ"])
        CP("dve", widx_i, cmpJ[:, 0, 0:NOT], r=["cmpJ"], w=["widx_i"])
        TS1("dve", eC, iota_f, float(CAP), ALU.mult, r=["iota_f"], w=["eC"])
        TT("dve", Bv, obase, eC, ALU.subtract, r=["obase", "eC"], w=["Bv"])
        TS1("dve", Bv, Bv, float(NE * CAP - CAP), ALU.add, r=["Bv"], w=["Bv"])
        TS1("dve", isov, rank, float(CAP), ALU.is_ge, r=["rank"], w=["isov"])
        TT("dve", isov, isov, Bv.unsqueeze(1).to_broadcast([128, NT, NE]), ALU.mult, r=["isov", "Bv"], w=["isov"])
        TT("dve", sval, rank, eC.unsqueeze(1).to_broadcast([128, NT, NE]), ALU.add, r=["rank", "eC"], w=["sval"])
        TT("dve", sval, sval, isov, ALU.add, r=["sval", "isov"], w=["sval"])
        TT("dve", isov, mk1, sval, ALU.mult, r=["mk1", "sval"], w=["isov"])
        P.add("dve", lambda e: e.tensor_reduce(out=slots_f[:, :, 0], in_=isov, axis=AX.X, op=ALU.add), r=["isov"], w=["slots_f"])
        TT("dve", isov, mk2, sval, ALU.mult, r=["mk2", "sval", "slots_f"], w=["isov"])
        P.add("dve", lambda e: e.tensor_reduce(out=slots_f[:, :, 1], in_=isov, axis=AX.X, op=ALU.add), r=["isov"], w=["slots_f"])
        CP("dve", slots_i, slots_f, r=["slots_f"], w=["slots_i"])
        DBG("slots", slots_f.rearrange("p a b -> p (a b)"), [128, NT * 2], r=["slots_f"])
        DBG("oe", oe_f, [128, NOT], r=["oe_f"])
        DBG("wt1", wt1, [128, NT], r=["wt1"])
        DBG("wt2", wt2, [128, NT], r=["wt2"])

        scat_names = []
        for i in range(NT):
            for k in range(2):
                nm = f"Xs{i}_{k}"
                scat_names.append(nm)
                P.add("pool", (lambda i=i, k=k: (lambda e: e.indirect_dma_start(
                    out=X_scr[:, :], out_offset=bass.IndirectOffsetOnAxis(ap=slots_i[:, i, k:k + 1], axis=0),
                    in_=u2tm[:, i, :], in_offset=None, bounds_check=rb_slot, oob_is_err=False)))(),
                    r=["slots_i", "u2tm"], w=[nm], dsem="scat")

        for ws0 in range(1, NWS):
            load_weights(ws0)

        NXB, NYB, LA = 6, 4, 5
        tiles = []
        jo = 0
        for ex in range(NE):
            for ti in range(CAP // 128):
                tiles.append((ex, ex * CAP + ti * 128))
            if ex >= 1 and jo < NOT:
                tiles.append((NE + jo, NE * CAP + jo * 128))
                jo += 1
        while jo < NOT:
            tiles.append((NE + jo, NE * CAP + jo * 128))
            jo += 1
        NTL = len(tiles)
        P.barrier(keep=("wE1", "wE2"))
        A.release(route_mark)
        Xt = [A.alloc([128, 1024], BF16) for _ in range(NXB)]
        xT = [A.alloc([128, 8, 128], BF16) for _ in range(NXB)]
        stmp = [A.alloc([128, 512], BF16) for _ in range(2)]
        hid = [A.alloc([128, 512], BF16) for _ in range(2)]
        hidT = [A.alloc([128, 4, 128], BF16) for _ in range(2)]
        Yt = [A.alloc([128, 1024], F32) for _ in range(NYB)]
        scat_names = []

        def stageA1(n):
            ws, row = tiles[n]
            b3 = n % NXB
            DMA("sp", Xt[b3], X_scr[row:row + 128, :], r=scat_names, w=[f"Xt{b3}"], dsem=f"Xt{b3}")

        def stageA(n):
            b3 = n % NXB
            b = P.nb()
            pT = bankb(b).rearrange("p (a b) -> p a b", a=8)
            TR([(pT[:, kc, :], Xt[b3][:, kc * 128:(kc + 1) * 128]) for kc in range(8)], identb, r=[f"Xt{b3}", "identb"], w=[f"ps{b}"])
            ACT(xT[b3], pT, AF.Copy, r=[f"ps{b}"], w=[f"xT{b3}"])

        def stageB(n):
            ws, row = tiles[n]
            load_weights(ws)
            s_ = wslot(ws)
            W1, W3, W2 = wE[s_]
            b3, b2 = n % NXB, n % 2
            b1_, b3_ = P.nb(), P.nb()
            MM(bankf(b1_), [(xT[b3][:, kc, :], W1[:, kc, :]) for kc in range(8)], r=[f"xT{b3}", f"wE{s_}"], w=[f"ps{b1_}"])
            MM(bankf(b3_), [(xT[b3][:, kc, :], W3[:, kc, :]) for kc in range(8)], r=[f"xT{b3}", f"wE{s_}"], w=[f"ps{b3_}"])
            ACT(stmp[b2], bankf(b1_), AF.Silu, r=[f"ps{b1_}"], w=[f"stmp{b2}"])
            TT("dve", hid[b2], bankf(b3_), stmp[b2], ALU.mult, r=[f"ps{b3_}", f"stmp{b2}"], w=[f"hid{b2}"])

        def stageC(n):
            b2 = n % 2
            b = P.nb()
            pT = bankb(b)[:, 0:512].rearrange("p (a b) -> p a b", a=4)
            TR([(pT[:, fc, :], hid[b2][:, fc * 128:(fc + 1) * 128]) for fc in range(4)], identb, r=[f"hid{b2}", "identb"], w=[f"ps{b}"])
            CP("dve", hidT[b2], pT, r=[f"ps{b}"], w=[f"hidT{b2}"])

        def stageD(n):
            ws, row = tiles[n]
            s_ = wslot(ws)
            W2 = wE[s_][2]
            b2 = n % 2
            by = n % NYB
            for hf in range(2):
                b = P.nb()
                MM(bankf(b), [(hidT[b2][:, fc, :], W2[:, fc, hf * 512:(hf + 1) * 512]) for fc in range(4)],
                   r=[f"hidT{b2}", f"wE{s_}"], w=[f"ps{b}"])
                TT("dve", Yt[by][:, hf * 512:(hf + 1) * 512], bankf(b), mod(5, hf * 512, (hf + 1) * 512), ALU.mult,
                   r=[f"ps{b}"], w=[f"Yt{by}"])
            DMA("sp", Y_scr[row:row + 128, :], Yt[by], r=[f"Yt{by}"], w=[f"Yw{n}"], dsem=f"yout{by}")

        load_weights(NE + 0)
        for n0 in range(LA):
            stageA1(n0)
        stageA(0)
        stageA(1)
        stageB(0)
        for n in range(NTL):
            stageC(n)
            if n + 1 < NTL:
                stageB(n + 1)
            if n + LA < NTL:
                stageA1(n + LA)
            if n + 2 < NTL:
                stageA(n + 2)
            stageD(n)
            if n + 1 < NTL:
                ws1 = tiles[n + 1][0]
                if ws1 < NE and tiles[n][0] != ws1:
                    if 1 <= ws1 < NOT:
                        load_weights(NE + ws1)
                    if ws1 + 2 < NE:
                        load_weights(ws1 + 2)

        P.barrier()
        A.release(moe_tmp_mark)
        NCB = 3
        hf_t = [A.alloc([128, 1024], F32) for _ in range(NCB)]
        o_t = [A.alloc([128, 1024], F32) for _ in range(2)]
        g1_t = [A.alloc([128, 1024], F32) for _ in range(NCB)]
        g2_t = [A.alloc([128, 1024], F32) for _ in range(NCB)]
        tmpB = [A.alloc([128, 1024], F32) for _ in range(2)]
        junk2 = A.alloc([128, 1024], BF16)
        def HLOAD(gt):
            p3 = gt % NCB
            DMA("sp", hf_t[p3], h_scr[gt * 128:(gt + 1) * 128, :], r=[], w=[f"hf{p3}"], dsem=f"hf{p3}")

        for gt in range(NCB):
            HLOAD(gt)

        def C_front(gt):
            p3 = gt % NCB
            hh = hf_t[p3]
            for (k, gbuf, nm) in ((0, g1_t[p3], f"g1_{p3}"), (1, g2_t[p3], f"g2_{p3}")):
                P.add("pool", (lambda gt=gt, k=k, gbuf=gbuf: (lambda e: e.indirect_dma_start(
                    out=gbuf, out_offset=None, in_=Y_scr[:, :],
                    in_offset=bass.IndirectOffsetOnAxis(ap=slots_i[:, gt, k:k + 1], axis=0),
                    bounds_check=rb_slot, oob_is_err=False)))(), r=[], w=[nm], dsem=nm)
            STT("dve", hh, g1_t[p3], wt1[:, gt:gt + 1], hh, ALU.mult, ALU.add, r=[f"g1_{p3}", f"hf{p3}"], w=[f"hf{p3}"])
            STT("dve", hh, g2_t[p3], wt2[:, gt:gt + 1], hh, ALU.mult, ALU.add, r=[f"g2_{p3}", f"hf{p3}"], w=[f"hf{p3}"])
            ACT(junk2, hh, AF.Square, r=[f"hf{p3}"], w=["junk2", f"ss3_{gt}"], accum_out=ss3[:, gt:gt + 1])
            RSTD(rstd3[:, gt:gt + 1], ss3[:, gt:gt + 1], 1024.0, r=[f"ss3_{gt}"], w=[f"rstd3_{gt}"])

        def C_back(gt):
            p2 = gt % 2
            p3 = gt % NCB
            hh = hf_t[p3]
            oo = o_t[p2]
            tb = tmpB[p2]
            STT("dve", tb, hh, rstd3[:, gt:gt + 1], mod(7), ALU.mult, ALU.mult, r=[f"hf{p3}", f"rstd3_{gt}"], w=[f"tmpB{p2}"])
            TT("dve", oo, tb, mod(6), ALU.add, r=[f"tmpB{p2}"], w=[f"oo{p2}"])
            DMA("sp", out[gt * 128:(gt + 1) * 128, :], oo, r=[f"oo{p2}"], w=[f"out{gt}"], dsem=f"out{p2}")
            if gt + NCB < NT:
                HLOAD(gt + NCB)

        C_front(0)
        for gt in range(NT):
            if gt + 1 < NT:
                C_front(gt + 1)
            C_back(gt)
        P.final.append(("sp", "out0"))
        P.final.append(("sp", "out1"))
        if "dbg" in P.dsems:
            P.final.append(("pool", "dbg"))
        for nm_ in ("hout0", "hout1", "u2out0", "u2out1"):
            P.final.append(("sp", nm_))

        block = es.enter_context(nc.Block())
        P.emit(block)
    return nc, dbg_outs


def pack_experts(w1, w3, w2):
    w1 = np.asarray(w1, dtype=np.float32).reshape(NE, 8, 128, 512).transpose(0, 2, 1, 3).reshape(NE, 128, 4096)
    w3 = np.asarray(w3, dtype=np.float32).reshape(NE, 8, 128, 512).transpose(0, 2, 1, 3).reshape(NE, 128, 4096)
    w2 = np.asarray(w2, dtype=np.float32).reshape(NE, 4, 128, 1024).transpose(0, 2, 1, 3).reshape(NE, 128, 4096)
    return np.ascontiguousarray(np.concatenate([w1, w3, w2], axis=2).reshape(NE * 128, 12288))


def make_in_map(inputs, b):
    f = lambda a: np.ascontiguousarray(np.asarray(a, dtype=np.float32))
    i = inputs
    return {
        "x": f(i["x"][b]),
        "c": f(i["c"][b].reshape(8, 128)),
        "w_ada": f(i["w_ada"][0]),
        "b_ada": f(i["b_ada"][0].reshape(1, 6144)),
        "g_norm1": f(i["g_norm1"][0].reshape(1, 1024)),
        "w_in": f(i["w_in"][0]),
        "w_dw": f(i["w_dw"][0].reshape(31, 512)),
        "b_dw": f(i["b_dw"][0].reshape(4, 128)),
        "g_conv_ln": f(i["g_conv_ln"][0].reshape(4, 128)),
        "b_conv_ln": f(i["b_conv_ln"][0].reshape(4, 128)),
        "w_conv_pw": f(i["w_conv_pw"][0]),
        "b_conv_pw": f(i["b_conv_pw"][0].reshape(8, 128)),
        "w_a2": f(i["w_a2"][0]),
        "b_a2": f(i["b_a2"][0].reshape(1, 512)),
        "g_gla_norm": f(i["g_gla_norm"][0].reshape(1, 1024)),
        "w_gla_o": f(i["w_gla_o"][0]),
        "w_out": f(i["w_out"][0]),
        "g_norm2": f(i["g_norm2"][0].reshape(1, 1024)),
        "w_router_g": f(i["w_router_g"][0]),
        "b_router_g": f(i["b_router_g"][0].reshape(1, 4)),
        "w_router_e": f(i["w_router_e"][0]),
        "b_router_e": f(i["b_router_e"][0].reshape(1, 32)),
        "wpack": pack_experts(i["w1"][0], i["w3"][0], i["w2"][0]),
        "w_ada_f": f(i["w_ada_f"]),
        "b_ada_f": f(i["b_ada_f"].reshape(1, 2048)),
        "g_final": f(i["g_final"].reshape(1, 1024)),
    }


_NC_CACHE = {}


def kernel(**inputs):
    if "nc" not in _NC_CACHE:
        _NC_CACHE["nc"] = build_program()[0]
    nc = _NC_CACHE["nc"]
    shared = make_in_map(inputs, 0)
    in_maps = []
    for b in range(8):
        m = dict(shared)
        m["x"] = np.ascontiguousarray(np.asarray(inputs["x"][b], dtype=np.float32))
        m["c"] = np.ascontiguousarray(np.asarray(inputs["c"][b], dtype=np.float32).reshape(8, 128))
        in_maps.append(m)
    res = run_bass_kernel_spmd(nc, in_maps, core_ids=list(range(8)))
    return np.stack([np.asarray(r["out"], dtype=np.float32) for r in res.results], axis=0)
```
